# Optimizing a Trainium2 kernel written in Bass

```python
import jax, jax.numpy as jnp
from jax import lax
import numpy as np

D_MODEL = 1024
BATCH = 4
SEQ = 4096
DEPTH = 1

CHUNK = 64
PLE_DIM = 256
EPS = 1e-6

GLA_HEADS = 4
GLA_DK = 128
GLA_DV = 256
GLA_GATE_RANK = 16
GLA_TAU = 16.0

SB_HEADS = 16
SB_DH = 64
SB_BLOCK = 128

PEER_HEADS = 8
PEER_NKEYS = 128
PEER_N = PEER_NKEYS * PEER_NKEYS
PEER_DKEY = 256
PEER_TOPK = 16
PEER_TOK_BLOCK = 128

GLA_QK = GLA_HEADS * GLA_DK
GLA_V = GLA_HEADS * GLA_DV
SB_W = SB_HEADS * SB_DH
IN_SPLITS = (GLA_QK, GLA_QK, GLA_V, GLA_V, GLA_GATE_RANK, SB_W, SB_W, SB_W, D_MODEL, D_MODEL)
IN_WIDTH = sum(IN_SPLITS)

kernel_name = "hybrid_gla_stickbreak_peer_block"


def rmsnorm(x, g):
    xf = x.astype(jnp.float32)
    y = xf * lax.rsqrt(jnp.mean(xf * xf, axis=-1, keepdims=True) + EPS)
    return (y * g.astype(jnp.float32)).astype(x.dtype)


def gla_branch(q, k, v, r, a_lo, w_a_up, b_a, g_o):
    B, S, _ = q.shape
    NC = S // CHUNK
    f32 = jnp.float32
    qf = q.astype(f32).reshape(B, NC, CHUNK, GLA_HEADS, GLA_DK) * (GLA_DK ** -0.5)
    kf = k.astype(f32).reshape(B, NC, CHUNK, GLA_HEADS, GLA_DK)
    vf = v.astype(f32).reshape(B, NC, CHUNK, GLA_HEADS, GLA_DV)
    log_a = jax.nn.log_sigmoid((a_lo @ w_a_up + b_a).astype(f32)) / GLA_TAU
    log_a = log_a.reshape(B, NC, CHUNK, GLA_HEADS, GLA_DK)
    logb = jnp.cumsum(log_a, axis=2)
    k_dec = kf * jnp.exp(logb[:, :, -1:] - logb)
    g_chunk = jnp.exp(logb[:, :, -1])

    def step(state, inp):
        qc, kc, vc, gc = inp
        state = gc[..., None] * state + jnp.einsum('bchk,bchv->bhkv', kc, vc)
        oc = jnp.einsum('bchk,bhkv->bchv', qc, state)
        return state, oc

    xs = (jnp.moveaxis(qf, 1, 0), jnp.moveaxis(k_dec, 1, 0),
          jnp.moveaxis(vf, 1, 0), jnp.moveaxis(g_chunk, 1, 0))
    state0 = jnp.zeros((B, GLA_HEADS, GLA_DK, GLA_DV), f32)
    _, o = lax.scan(step, state0, xs)
    o = jnp.moveaxis(o, 0, 1).reshape(B, S, GLA_HEADS, GLA_DV)
    o = o * lax.rsqrt(jnp.mean(o * o, axis=-1, keepdims=True) + EPS)
    o = o * g_o.astype(f32).reshape(GLA_HEADS, GLA_DV)
    o = o.reshape(B, S, GLA_V) * jax.nn.silu(r.astype(f32))
    return o.astype(q.dtype)


def stick_breaking_branch(q, k, v):
    B, S, _ = q.shape
    NB = S // SB_BLOCK
    f32 = jnp.float32
    qf = q.astype(f32).reshape(B, S, SB_HEADS, SB_DH).transpose(0, 2, 1, 3) * (SB_DH ** -0.5)
    kf = k.astype(f32).reshape(B, S, SB_HEADS, SB_DH).transpose(0, 2, 1, 3)
    vf = v.astype(f32).reshape(B, S, SB_HEADS, SB_DH).transpose(0, 2, 1, 3)
    qb = qf.reshape(B, SB_HEADS, NB, SB_BLOCK, SB_DH).transpose(2, 0, 1, 3, 4)
    starts = jnp.arange(NB, dtype=jnp.int32) * SB_BLOCK
    key_pos = jnp.arange(S, dtype=jnp.int32)

    def block(args):
        qblk, start = args
        z = jnp.einsum('bhqd,bhkd->bhqk', qblk, kf)
        q_pos = start + jnp.arange(SB_BLOCK, dtype=jnp.int32)
        mask = key_pos[None, :] < q_pos[:, None]
        log_beta = jax.nn.log_sigmoid(z)
        log_stay = jnp.where(mask, jax.nn.log_sigmoid(-z), 0.0)
        after = lax.cumsum(log_stay, axis=3, reverse=True) - log_stay
        w = jnp.where(mask, jnp.exp(log_beta + after), 0.0)
        return jnp.einsum('bhqk,bhkd->bhqd', w, vf)

    o = lax.map(block, (qb, starts))
    o = o.transpose(1, 0, 3, 2, 4).reshape(B, S, SB_W)
    return o.astype(q.dtype)


def peer_ffn(x, w_pq, sub_k1, sub_k2, u_emb, v_emb):
    B, S, D = x.shape
    T = B * S
    half = PEER_DKEY // 2
    xt = x.reshape(T, D)
    q = (xt @ w_pq).astype(jnp.float32).reshape(T, PEER_HEADS, PEER_DKEY)
    s1 = jnp.einsum('thd,hnd->thn', q[..., :half], sub_k1.astype(jnp.float32))
    s2 = jnp.einsum('thd,hnd->thn', q[..., half:], sub_k2.astype(jnp.float32))
    v1, i1 = lax.top_k(s1, PEER_TOPK)
    v2, i2 = lax.top_k(s2, PEER_TOPK)
    cand = (v1[..., :, None] + v2[..., None, :]).reshape(T, PEER_HEADS, PEER_TOPK * PEER_TOPK)
    cand_idx = (i1[..., :, None] * PEER_NKEYS + i2[..., None, :]).reshape(T, PEER_HEADS, PEER_TOPK * PEER_TOPK)
    top_s, top_c = lax.top_k(cand, PEER_TOPK)
    expert = jnp.take_along_axis(cand_idx, top_c, axis=-1)
    gate = jax.nn.softmax(top_s, axis=-1)

    NB = T // PEER_TOK_BLOCK
    K = PEER_HEADS * PEER_TOPK
    xb = xt.reshape(NB, PEER_TOK_BLOCK, D)
    eb = expert.reshape(NB, PEER_TOK_BLOCK, K)
    gb = gate.reshape(NB, PEER_TOK_BLOCK, K)

    def block(args):
        xblk, eblk, gblk = args
        u = jnp.take(u_emb, eblk, axis=0)
        h = jax.nn.gelu(jnp.einsum('tkd,td->tk', u, xblk).astype(jnp.float32))
        a = (h * gblk).astype(xblk.dtype)
        vv = jnp.take(v_emb, eblk, axis=0)
        return jnp.einsum('tk,tkd->td', a, vv).astype(xblk.dtype)

    out = lax.map(block, (xb, eb, gb))
    return out.reshape(B, S, D)


def setup_inputs(seed: int = 0) -> dict:
    key = jax.random.key(seed)
    ks = jax.random.split(key, 24)
    n = jax.random.normal
    f = jnp.float32
    L = DEPTH
    return {
        "x": n(ks[0], (BATCH, SEQ, D_MODEL), f),
        "p": n(ks[1], (L, BATCH, SEQ, PLE_DIM), f),
        "g_mix": 1.0 + 0.01 * n(ks[2], (L, D_MODEL), f),
        "w_in": n(ks[3], (L, D_MODEL, IN_WIDTH), f) * D_MODEL ** -0.5,
        "w_gla_a_up": n(ks[4], (L, GLA_GATE_RANK, GLA_QK), f) * GLA_GATE_RANK ** -0.5,
        "b_gla_a": 0.1 * n(ks[5], (L, GLA_QK), f),
        "g_gla_o": 1.0 + 0.01 * n(ks[6], (L, GLA_V), f),
        "w_gla_out": n(ks[7], (L, GLA_V, D_MODEL), f) * GLA_V ** -0.5,
        "w_sb_out": n(ks[8], (L, SB_W, D_MODEL), f) * SB_W ** -0.5,
        "w_o": n(ks[9], (L, D_MODEL, D_MODEL), f) * D_MODEL ** -0.5,
        "g_ffn": 1.0 + 0.01 * n(ks[10], (L, D_MODEL), f),
        "w_peer_q": n(ks[11], (L, D_MODEL, PEER_HEADS * PEER_DKEY), f) * D_MODEL ** -0.5,
        "peer_k1": n(ks[12], (L, PEER_HEADS, PEER_NKEYS, PEER_DKEY // 2), f) * (PEER_DKEY // 2) ** -0.5,
        "peer_k2": n(ks[13], (L, PEER_HEADS, PEER_NKEYS, PEER_DKEY // 2), f) * (PEER_DKEY // 2) ** -0.5,
        "peer_u": n(ks[14], (L, PEER_N, D_MODEL), f) * D_MODEL ** -0.5,
        "peer_v": n(ks[15], (L, PEER_N, D_MODEL), f) * PEER_HEADS ** -0.5,
        "g_ple": 1.0 + 0.01 * n(ks[16], (L, D_MODEL), f),
        "w_ple_gate": n(ks[17], (L, D_MODEL, D_MODEL), f) * D_MODEL ** -0.5,
        "w_ple": n(ks[18], (L, PLE_DIM, D_MODEL), f) * PLE_DIM ** -0.5,
        "g_final": 1.0 + 0.01 * n(ks[19], (D_MODEL,), f),
    }


def reference(x, p, g_mix, w_in, w_gla_a_up, b_gla_a, g_gla_o, w_gla_out, w_sb_out, w_o,
              g_ffn, w_peer_q, peer_k1, peer_k2, peer_u, peer_v, g_ple, w_ple_gate, w_ple, g_final):
    offsets = [int(o) for o in np.cumsum(IN_SPLITS)[:-1]]
    h = x
    for i in range(DEPTH):
        u = rmsnorm(h, g_mix[i])
        proj = u @ w_in[i]
        (gq, gk, gv, gr, ga, sq, sk, sv, gate_a, gate_b) = jnp.split(proj, offsets, axis=-1)
        y_a = gla_branch(gq, gk, gv, gr, ga, w_gla_a_up[i], b_gla_a[i], g_gla_o[i]) @ w_gla_out[i]
        y_b = stick_breaking_branch(sq, sk, sv) @ w_sb_out[i]
        mixed = jax.nn.sigmoid(gate_a) * y_a + jax.nn.sigmoid(gate_b) * y_b
        h = h + (mixed @ w_o[i]).astype(h.dtype)
        h = h + peer_ffn(rmsnorm(h, g_ffn[i]), w_peer_q[i], peer_k1[i], peer_k2[i],
                         peer_u[i], peer_v[i]).astype(h.dtype)
        ple = p[i] @ w_ple[i]
        ple_gate = jax.nn.sigmoid(rmsnorm(h, g_ple[i]) @ w_ple_gate[i])
        h = h + (ple * ple_gate).astype(h.dtype)
    return rmsnorm(h, g_final)
```

```python
import numpy as np
import ml_dtypes
import concourse.bass as bass
import concourse.mybir as mybir
from concourse.bass_utils import run_bass_kernel_spmd

F32 = mybir.dt.float32
BF16 = mybir.dt.bfloat16
I32 = mybir.dt.int32
U32 = mybir.dt.uint32
AF = mybir.ActivationFunctionType
ALU = mybir.AluOpType

EPS = 1e-6
SB_BASE = 16512
SB_LIMIT = 229344


class Buf:
    __slots__ = ("w", "rs", "name")

    def __init__(self, name=""):
        self.w = None
        self.rs = []
        self.name = name


class Sched:
    EPOCH = 4096
    NDMA = {"sp": 24, "pool": 24, "act": 8}

    def __init__(self, nc):
        self.nc = nc
        self.names = ["pe", "act", "dve", "pool", "sp"]
        self.ops = {n: [] for n in self.names}
        self.count = {n: 0 for n in self.names}
        self.esems = {n: [] for n in self.names}
        self.waited = {n: {} for n in self.names}
        self.dsems = {q: [] for q in self.NDMA}
        self.duse = {q: [] for q in self.NDMA}
        self.dnext = {q: 0 for q in self.NDMA}
        self.semctx = []

    def _newsem(self, name):
        cm = self.nc.semaphore(name)
        s = cm.__enter__()
        self.semctx.append(cm)
        return s

    def _tok_compute(self, eng):
        n = self.count[eng]
        ep, idx = divmod(n, self.EPOCH)
        while len(self.esems[eng]) <= ep:
            self.esems[eng].append(self._newsem(f"e_{eng}_{len(self.esems[eng])}"))
        self.count[eng] = n + 1
        return (self.esems[eng][ep], idx + 1)

    def _deps(self, r, w):
        deps = []
        for b in r:
            if b.w is not None:
                deps.append(b.w)
        for b in w:
            if b.w is not None:
                deps.append(b.w)
            deps.extend(b.rs)
        return deps

    def _commit(self, tok, r, w):
        for b in r:
            b.rs.append(tok)
        for b in w:
            b.w = tok
            b.rs = []

    def _waits(self, eng, deps):
        best = {}
        for (s, v) in deps:
            k = id(s)
            if k not in best or best[k][1] < v:
                best[k] = (s, v)
        out = []
        wd = self.waited[eng]
        for k, (s, v) in best.items():
            if wd.get(k, 0) >= v:
                continue
            wd[k] = v
            out.append((s, v))
        return out

    def op(self, eng, fn, r=(), w=()):
        waits = self._waits(eng, self._deps(r, w))
        tok = self._tok_compute(eng)
        self.ops[eng].append((waits, fn, (tok[0], 1)))
        self._commit(tok, r, w)
        return tok

    def dma(self, q, fn, r=(), w=()):
        if len(self.dsems[q]) < self.NDMA[q]:
            self.dsems[q].append(self._newsem(f"d_{q}_{len(self.dsems[q])}"))
            self.duse[q].append(0)
        i = self.dnext[q]
        self.dnext[q] = (i + 1) % self.NDMA[q]
        s = self.dsems[q][i]
        deps = self._deps(r, w)
        if self.duse[q][i] > 0:
            deps.append((s, 16 * self.duse[q][i]))
        waits = self._waits(q, deps)
        self.duse[q][i] += 1
        tok = (s, 16 * self.duse[q][i])
        self.ops[q].append((waits, fn, (s, 16)))
        self._commit(tok, r, w)
        return tok

    def _all_tokens(self):
        deps = []
        for n in self.names:
            c = self.count[n]
            if c > 0:
                ep, idx = divmod(c - 1, self.EPOCH)
                deps.append((self.esems[n][ep], idx + 1))
        for q in self.NDMA:
            for s, u in zip(self.dsems[q], self.duse[q]):
                if u > 0:
                    deps.append((s, 16 * u))
        return deps

    def barrier(self, engines=None):
        deps = self._all_tokens()
        for n in (engines or self.names):
            waits = self._waits(n, list(deps))
            if waits:
                self.ops[n].append((waits, None, None))

    def emit(self):
        nc = self.nc
        ops = self.ops

        def replay(e, lst):
            for waits, fn, inc in lst:
                for (s, v) in waits:
                    e.wait_ge(s, v)
                if fn is None:
                    continue
                ins = fn(e)
                ins.then_inc(inc[0], inc[1])

        with nc.Block() as block:
            @block.tensor
            def _(e):
                replay(e, ops["pe"])

            @block.scalar
            def _(e):
                replay(e, ops["act"])

            @block.vector
            def _(e):
                replay(e, ops["dve"])

            @block.gpsimd
            def _(e):
                replay(e, ops["pool"])

            @block.sync
            def _(e):
                replay(e, ops["sp"])


class Tl:
    __slots__ = ("t", "b")

    def __init__(self, t, name=""):
        self.t = t
        self.b = Buf(name)


class Alloc:
    def __init__(self, nc):
        self.nc = nc
        self.off = SB_BASE
        self.n = 0

    def mark(self):
        return self.off

    def reset(self, m):
        self.off = m

    def tile(self, shape, dt, name="t"):
        esz = {F32: 4, BF16: 2, I32: 4, U32: 4}[dt]
        nbytes = int(np.prod(shape[1:])) * esz
        nbytes = (nbytes + 31) // 32 * 32
        assert self.off + nbytes <= SB_LIMIT, f"SBUF overflow at {name}: {self.off}+{nbytes}"
        self.n += 1
        t = self.nc.alloc_sbuf_tensor_at(f"{name}_{self.n}", list(shape), dt, offset=self.off)
        self.off += nbytes
        return Tl(t, name)

    def tiles(self, k, shape, dt, name="t"):
        return [self.tile(shape, dt, f"{name}{i}") for i in range(k)]


C_GQ, C_GK, C_GV, C_GR, C_GA, C_SQ, C_SK, C_SV, C_GTA, C_GTB = 0, 512, 1024, 2048, 3072, 3088, 4112, 5136, 6160, 7184


def build(debug=False):
    nc = bass.Bass("TRN2", target_bir_lowering=False)

    def din(name, shape, dt=F32):
        return nc.dram_tensor(name, list(shape), dt, kind="ExternalInput").ap()

    def dscr(name, shape, dt=BF16):
        return nc.dram_tensor(name, list(shape), dt, kind="Internal").ap()

    xT_true = din("xT_true", [1024, 4096])
    xT_own = din("xT_own", [1024, 2048])
    masks_d = din("masks", [128, 16 * 512], BF16)
    x_own = din("x_own", [2048, 1024])
    pT_own = din("pT_own", [256, 2048])
    flags_d = din("flags", [128, 8])
    w_in = din("w_in", [1024, 8208])
    w_a_up = din("w_a_up", [16, 512])
    b_a_bc = din("b_a_bc", [128, 512])
    g_mix_pk = din("g_mix_pk", [128, 8])
    g_o_bc = din("g_o_bc", [128, 1024])
    w_gla_out = din("w_gla_out", [1024, 1024])
    w_sb_out = din("w_sb_out", [1024, 1024])
    w_o = din("w_o", [1024, 1024])
    g_ffn_bc = din("g_ffn_bc", [128, 1024])
    w_pq = din("w_pq", [1024, 2048])
    k1T = din("k1T", [128, 1024])
    k2T = din("k2T", [128, 1024])
    peer_uv = din("peer_uv", [16384, 2048])
    g_ple_bc = din("g_ple_bc", [128, 1024])
    w_pg = din("w_pg", [1024, 1024])
    w_ple = din("w_ple", [256, 1024])
    g_fin_bc = din("g_fin_bc", [128, 1024])
    consts_d = din("consts", [128, 6 * 128 + 2 + 16])
    out_d = nc.dram_tensor("out", [2048, 1024], F32, kind="ExternalOutput").ap()

    s_kg = dscr("s_kg", [4096, 512])
    s_vg = dscr("s_vg", [4096, 1024])
    s_aT = dscr("s_aT", [16, 4096], F32)
    s_qg = dscr("s_qg", [512, 2048])
    s_rg = dscr("s_rg", [2048, 1024])
    s_ksb = dscr("s_ksb", [1024, 4096])
    s_vsb = dscr("s_vsb", [4096, 1024])
    s_qsb = dscr("s_qsb", [1024, 2048])
    s_sga = dscr("s_sga", [1024, 2048])
    s_sgb = dscr("s_sgb", [1024, 2048])
    s_wga = dscr("s_wga", [1024, 1024])
    s_wsb = dscr("s_wsb", [1024, 1024])
    s_wo = dscr("s_wo", [1024, 1024])
    s_wpg = dscr("s_wpg", [1024, 1024])
    s_wpq = dscr("s_wpq", [1024, 2048])
    s_wpl = dscr("s_wpl", [256, 1024])
    s_k1 = dscr("s_k1", [128, 1024])
    s_k2 = dscr("s_k2", [128, 1024])
    s_pT = dscr("s_pT", [256, 2048])
    s_uvb = dscr("s_uvb", [16384, 2048])

    dbg = {}
    if debug:
        dbg["h1"] = nc.dram_tensor("dbg_h1", [2048, 1024], F32, kind="ExternalOutput").ap()
        dbg["ogT"] = nc.dram_tensor("dbg_ogT", [1024, 2048], BF16, kind="ExternalOutput").ap()
        dbg["osbT"] = nc.dram_tensor("dbg_osbT", [1024, 2048], BF16, kind="ExternalOutput").ap()
        dbg["h2"] = nc.dram_tensor("dbg_h2", [2048, 1024], F32, kind="ExternalOutput").ap()
        dbg["eid"] = nc.dram_tensor("dbg_eid", [2048, 128], I32, kind="ExternalOutput").ap()
        dbg["aT"] = nc.dram_tensor("dbg_aT", [16, 4096], F32, kind="ExternalOutput").ap()
        dbg["qg"] = nc.dram_tensor("dbg_qg", [512, 2048], BF16, kind="ExternalOutput").ap()
        dbg["rg"] = nc.dram_tensor("dbg_rg", [2048, 1024], BF16, kind="ExternalOutput").ap()
        dbg["kg"] = nc.dram_tensor("dbg_kg", [4096, 512], BF16, kind="ExternalOutput").ap()
        dbg["obuf"] = nc.dram_tensor("dbg_obuf", [128, 16 * 512], F32, kind="ExternalOutput").ap()
        dbg["gch"] = nc.dram_tensor("dbg_gch", [128, 128], F32, kind="ExternalOutput").ap()
        dbg["kdec"] = nc.dram_tensor("dbg_kdec", [128, 32 * 256], BF16, kind="ExternalOutput").ap()

    S = Sched(nc)
    A = Alloc(nc)
    psall = nc.alloc_psum_tensor("psall", [128, 4096], F32)
    ps = [Tl(psall[:, i * 512:(i + 1) * 512], f"ps{i}") for i in range(8)]

    cf = A.tile([128, 6 * 128 + 2 + 16], F32, "cf")
    cb = A.tile([128, 5 * 128], BF16, "cb")
    flg = A.tile([128, 8], F32, "flg")
    S.dma("sp", lambda e: e.dma_start(out=cf.t[:], in_=consts_d[:, :]), w=[cf.b])
    S.dma("sp", lambda e: e.dma_start(out=flg.t[:], in_=flags_d[:, :]), w=[flg.b])
    S.op("act", lambda e: e.activation(out=cb.t[:], in_=cf.t[:, 128:768], func=AF.Copy), r=[cf.b], w=[cb.b])
    ident_f = cf.t[:, 0:128]
    ones_b = cb.t[:, 0:128]
    trisb_b = cb.t[:, 128:256]
    negones_b = cb.t[:, 256:384]
    trigla_f = cf.t[:, 512:640]
    trimask_f = cf.t[:, 640:768]
    chind_f = cf.t[:, 768:770]
    iota16_f = cf.t[:, 770:786]
    iota16_bt = A.tile([128, 16], BF16, "iota16b")
    S.op("act", lambda e: e.activation(out=iota16_bt.t[:], in_=iota16_f, func=AF.Copy), r=[cf.b], w=[iota16_bt.b])
    pmark = A.mark()

    cvstate = {"n": 0}
    uv_src = peer_uv.rearrange("(p r) n -> p r n", p=128)
    uv_dst = s_uvb.rearrange("(p r) n -> p r n", p=128)
    cvitems = []
    for (wsrc, wdst, rows, cols) in ((w_gla_out, s_wga, 1024, 1024), (w_sb_out, s_wsb, 1024, 1024), (w_o, s_wo, 1024, 1024),
                                     (w_pq, s_wpq, 1024, 2048), (k1T, s_k1, 128, 1024), (k2T, s_k2, 128, 1024),
                                     (w_pg, s_wpg, 1024, 1024), (w_ple, s_wpl, 256, 1024), (pT_own, s_pT, 256, 2048)):
        for r0 in range(0, rows, 128):
            for c0_ in range(0, cols, 1024):
                cvitems.append((wsrc[r0:r0 + 128, c0_:c0_ + 1024], wdst[r0:r0 + 128, c0_:c0_ + 1024]))
    for c in range(256):
        r_, hcol = c // 2, (c % 2) * 1024
        cvitems.append((uv_src[:, r_, hcol:hcol + 1024], uv_dst[:, r_, hcol:hcol + 1024]))
    NCV = len(cvitems)

    def make_converter(cvf, cvb, limit, pattern=("act", "dve")):
        nb_ = len(cvf)

        def convert_chunk():
            c = cvstate["n"]
            if c >= limit:
                return
            cvstate["n"] += 1
            src_, dst_ = cvitems[c]
            f_, b_ = cvf[c % nb_], cvb[c % nb_]
            S.dma("sp", lambda e: e.dma_start(out=f_.t[:], in_=src_), w=[f_.b])
            if pattern[c % len(pattern)] == "act":
                S.op("act", lambda e: e.activation(out=b_.t[:], in_=f_.t[:], func=AF.Copy), r=[f_.b], w=[b_.b])
            else:
                S.op("dve", lambda e: e.tensor_copy(out=b_.t[:], in_=f_.t[:]), r=[f_.b], w=[b_.b])
            S.dma("pool", lambda e: e.dma_start(out=dst_, in_=b_.t[:]), r=[b_.b], w=[])
        return convert_chunk

    def phaseA():
        uT = A.tile([128, 8, 4096], BF16, "uT")
        uTb = [Buf(f"uT{g}") for g in range(8)]
        xs = A.tiles(2, [128, 8, 512], F32, "xs")
        sq = A.tiles(2, [128, 8, 512], BF16, "sq")
        lnv = A.tiles(2, [128, 512], F32, "lnv")
        rstd = A.tiles(2, [128, 512], F32, "rstd")
        Wf = A.tiles(2, [128, 8, 512], F32, "Wf")
        Wb = A.tiles(2, [128, 8, 512], BF16, "Wb")
        ostg = A.tiles(4, [128, 512], BF16, "ostg")
        ostg_f = A.tile([16, 512], F32, "ostgf")
        gmix = A.tile([128, 8], F32, "gmix")
        S.dma("sp", lambda e: e.dma_start(out=gmix.t[:], in_=g_mix_pk[:, :]), w=[gmix.b])
        cnt = {"n": 0, "w": 0, "o": 0, "ps": 0}
        convert_chunk = make_converter([None], [None], 0)

        def norm(xT, ngroups):
            for g in range(ngroups):
                i = cnt["n"] % 2
                cnt["n"] += 1
                x_, sq_, ln_, rs_ = xs[i], sq[i], lnv[i], rstd[i]
                S.dma("sp", lambda e, x_=x_, g=g: e.dma_start(
                    out=x_.t[:], in_=xT[:, g * 512:(g + 1) * 512].rearrange("(kc p) t -> p kc t", p=128)), w=[x_.b])
                S.op("act", lambda e, x_=x_, sq_=sq_: e.activation(out=sq_.t[:], in_=x_.t[:], func=AF.Square),
                     r=[x_.b], w=[sq_.b])
                pb = ps[4 + (g % 2)]

                def mm(e, sq_=sq_, pb=pb):
                    for kc in range(8):
                        ins = e.matmul(pb.t[:, :], lhsT=ones_b, rhs=sq_.t[:, kc, :], start=(kc == 0), stop=(kc == 7))
                    return ins
                S.op("pe", mm, r=[sq_.b, cb.b], w=[pb.b])
                S.op("act", lambda e, ln_=ln_, pb=pb: e.activation(out=ln_.t[:], in_=pb.t[:, :], func=AF.Ln,
                                                                  bias=EPS, scale=1.0 / 1024.0), r=[pb.b], w=[ln_.b])
                S.op("act", lambda e, ln_=ln_, rs_=rs_: e.activation(out=rs_.t[:], in_=ln_.t[:], func=AF.Exp, scale=-0.5),
                     r=[ln_.b], w=[rs_.b])
                S.op("dve", lambda e, x_=x_, rs_=rs_, g=g: e.tensor_tensor(
                    out=uT.t[:, :, g * 512:(g + 1) * 512], in0=x_.t[:],
                    in1=rs_.t[:].unsqueeze(1).to_broadcast([128, 8, 512]), op=ALU.mult),
                    r=[x_.b, rs_.b], w=[uTb[g]])

        def load_w(c0, ncols):
            i = cnt["w"] % 2
            cnt["w"] += 1
            wf, wb = Wf[i], Wb[i]
            S.dma("sp", lambda e: e.dma_start(out=wf.t[:, :, 0:ncols],
                                              in_=w_in[:, c0:c0 + ncols].rearrange("(kc p) n -> p kc n", p=128)), w=[wf.b])
            S.op("dve", lambda e: e.tensor_tensor(out=wb.t[:, :, 0:ncols], in0=wf.t[:, :, 0:ncols],
                                                  in1=gmix.t[:].unsqueeze(2).to_broadcast([128, 8, ncols]), op=ALU.mult),
                 r=[wf.b, gmix.b], w=[wb.b])
            return wb

        def evac(pb, np_, ncols, func, scale, dst_ap_fn):
            o = ostg[cnt["o"] % 4]
            k = cnt["o"]
            cnt["o"] += 1
            if func is None and (k % 2 == 1):
                if scale == 1.0:
                    S.op("dve", lambda e: e.tensor_copy(out=o.t[0:np_, 0:ncols], in_=pb.t[0:np_, 0:ncols]), r=[pb.b], w=[o.b])
                else:
                    S.op("dve", lambda e: e.tensor_scalar(out=o.t[0:np_, 0:ncols], in0=pb.t[0:np_, 0:ncols], scalar1=scale,
                                                          scalar2=None, op0=ALU.mult), r=[pb.b], w=[o.b])
            else:
                f = AF.Copy if func is None else func
                S.op("act", lambda e: e.activation(out=o.t[0:np_, 0:ncols], in_=pb.t[0:np_, 0:ncols], func=f, scale=scale),
                     r=[pb.b], w=[o.b])
            S.dma("pool", lambda e: e.dma_start(out=dst_ap_fn(), in_=o.t[0:np_, 0:ncols]), r=[o.b], w=[])
            convert_chunk()

        def nextps():
            pb = ps[cnt["ps"] % 4]
            cnt["ps"] += 1
            return pb

        def gemm_tm(c0, ncols, own, func, scale, dst, dcol0):
            wb = load_w(c0, ncols)
            tiles = [(t, t) for t in range(16 if own else 32)]
            for (pt, dt_) in tiles:
                pb = nextps()

                def mm(e, pt=pt, pb=pb):
                    for kc in range(8):
                        ins = e.matmul(pb.t[:, 0:ncols], lhsT=uT.t[:, kc, pt * 128:(pt + 1) * 128], rhs=wb.t[:, kc, 0:ncols],
                                       start=(kc == 0), stop=(kc == 7))
                    return ins
                S.op("pe", mm, r=[uTb[pt // 4], wb.b], w=[pb.b])
                evac(pb, 128, ncols, func, scale, lambda dt_=dt_: dst[dt_ * 128:(dt_ + 1) * 128, dcol0:dcol0 + ncols])

        def gemm_fm(c0, ncols, own, func, scale, dst, drow0):
            wb = load_w(c0, ncols)
            groups = [(g, g) for g in range(4 if own else 8)]
            for nci in range(ncols // 128):
                for (pg, dg) in groups:
                    pb = nextps()

                    def mm(e, pg=pg, pb=pb, nci=nci):
                        for kc in range(8):
                            ins = e.matmul(pb.t[:, :], lhsT=wb.t[:, kc, nci * 128:(nci + 1) * 128],
                                           rhs=uT.t[:, kc, pg * 512:(pg + 1) * 512], start=(kc == 0), stop=(kc == 7))
                        return ins
                    S.op("pe", mm, r=[uTb[pg], wb.b], w=[pb.b])
                    evac(pb, 128, 512, func, scale,
                         lambda dg=dg, nci=nci: dst[drow0 + nci * 128: drow0 + (nci + 1) * 128, dg * 512:(dg + 1) * 512])

        norm(xT_true, 8)
        gemm_tm(C_GK, 512, False, None, 1.0, s_kg, 0)
        gemm_tm(C_GV, 512, False, None, 1.0, s_vg, 0)
        gemm_tm(C_GV + 512, 512, False, None, 1.0, s_vg, 512)
        wb = load_w(C_GA, 16)
        for g in range(8):
            pb = nextps()

            def mm(e, g=g, pb=pb, wb=wb):
                for kc in range(8):
                    ins = e.matmul(pb.t[0:16, :], lhsT=wb.t[:, kc, 0:16], rhs=uT.t[:, kc, g * 512:(g + 1) * 512],
                                   start=(kc == 0), stop=(kc == 7))
                return ins
            S.op("pe", mm, r=[uTb[g], wb.b], w=[pb.b])
            S.op("act", lambda e, pb=pb: e.activation(out=ostg_f.t[:, :], in_=pb.t[0:16, :], func=AF.Copy), r=[pb.b], w=[ostg_f.b])
            S.dma("pool", lambda e, g=g: e.dma_start(out=s_aT[:, g * 512:(g + 1) * 512], in_=ostg_f.t[:, :]), r=[ostg_f.b], w=[])
        for q in range(2):
            gemm_fm(C_SK + 512 * q, 512, False, None, 1.0, s_ksb, 512 * q)
            gemm_tm(C_SV + 512 * q, 512, False, None, 1.0, s_vsb, 512 * q)
        norm(xT_own, 4)
        gemm_fm(C_GQ, 512, True, None, 128.0 ** -0.5, s_qg, 0)
        for q in range(2):
            gemm_fm(C_SQ + 512 * q, 512, True, None, 0.125, s_qsb, 512 * q)
        for q in range(2):
            gemm_tm(C_GR + 512 * q, 512, True, AF.Silu, 1.0, s_rg, 512 * q)
        for q in range(2):
            gemm_fm(C_GTA + 512 * q, 512, True, AF.Sigmoid, 1.0, s_sga, 512 * q)
        for q in range(2):
            gemm_fm(C_GTB + 512 * q, 512, True, AF.Sigmoid, 1.0, s_sgb, 512 * q)

    phaseA()
    S.barrier()
    A.reset(pmark)

    ogT = A.tile([128, 8, 2048], BF16, "ogT")
    osbT = A.tile([128, 8, 2048], BF16, "osbT")
    ogTb = [[Buf() for _ in range(16)] for _ in range(8)]
    osbTb = [[Buf() for _ in range(4)] for _ in range(8)]
    pmark2 = A.mark()

    def phaseB():
        kk = A.tile([128, 32, 256], BF16, "kk")
        kkb = [Buf() for _ in range(32)]
        vv = A.tile([128, 32, 512], BF16, "vv")
        qT = A.tile([128, 2, 2048], BF16, "qTg")
        aT = A.tile([16, 4096], F32, "aT")
        wup = A.tile([16, 512], F32, "wup")
        bab = A.tile([128, 512], F32, "bab")
        gob = A.tile([128, 1024], F32, "gob")
        obuf = A.tile([128, 16, 512], F32, "obuf")
        obb = [Buf() for _ in range(16)]
        gch = A.tile([128, 32, 4], F32, "gch")
        gchb = [Buf() for _ in range(32)]
        zt = A.tiles(2, [128, 256], F32, "zt")
        et = A.tiles(2, [128, 256], F32, "et")
        spt = A.tiles(2, [128, 256], F32, "spt")
        edt = A.tiles(2, [128, 256], F32, "edt")
        st_f = A.tiles(2, [128, 256], F32, "stf")
        st_b = [A.tiles(2, [128, 256], BF16, f"stb{h}") for h in range(2)]
        rt = A.tiles(2, [128, 512], BF16, "rt")
        ssq = A.tiles(2, [128, 4], F32, "ssq")
        junk = A.tile([128, 256], F32, "junk")
        t1 = A.tiles(2, [128, 512], F32, "t1")
        og = A.tiles(2, [128, 512], F32, "og")
        convert_hook = make_converter([None], [None], 0)
        S.dma("sp", lambda e: e.dma_start(out=aT.t[:], in_=s_aT[:, :]), w=[aT.b])
        S.dma("sp", lambda e: e.dma_start(out=wup.t[:], in_=w_a_up[:, :]), w=[wup.b])
        S.dma("sp", lambda e: e.dma_start(out=bab.t[:], in_=b_a_bc[:, :]), w=[bab.b])
        S.dma("sp", lambda e: e.dma_start(out=gob.t[:], in_=g_o_bc[:, :]), w=[gob.b])
        def passB(hp):
            c0 = hp * 256
            for q4 in range(4):
                S.dma("sp", lambda e, q4=q4: e.dma_start(
                    out=kk.t[:, q4 * 8:(q4 + 1) * 8, :],
                    in_=s_kg[q4 * 1024:(q4 + 1) * 1024, c0:c0 + 256].rearrange("(t p) n -> p t n", p=128)),
                    w=[kkb[t] for t in range(q4 * 8, q4 * 8 + 8)])
            S.dma("sp", lambda e: e.dma_start(out=vv.t[:], in_=s_vg[:, hp * 512:(hp + 1) * 512].rearrange("(t p) n -> p t n", p=128)),
                  w=[vv.b])
            S.dma("sp", lambda e: e.dma_start(out=qT.t[:], in_=s_qg[hp * 256:(hp + 1) * 256, :].rearrange("(h p) t -> p h t", p=128)),
                  w=[qT.b])
            for tt in range(32):
                i = tt % 2
                z_, e_, sp_, ed_ = zt[i], et[i], spt[i], edt[i]
                pz = ps[i]
                S.op("pe", lambda e, tt=tt, pz=pz: e.matmul(pz.t[:, 0:256], lhsT=aT.t[:, tt * 128:(tt + 1) * 128],
                                                             rhs=wup.t[:, c0:c0 + 256], start=True, stop=True),
                     r=[aT.b, wup.b], w=[pz.b])
                S.op("dve", lambda e, z_=z_, pz=pz: e.tensor_tensor(out=z_.t[:], in0=pz.t[:, 0:256], in1=bab.t[:, c0:c0 + 256], op=ALU.add),
                     r=[pz.b, bab.b], w=[z_.b])
                S.op("act", lambda e, z_=z_, e_=e_: e.activation(out=e_.t[:], in_=z_.t[:], func=AF.Exp, scale=-1.0), r=[z_.b], w=[e_.b])
                S.op("act", lambda e, sp_=sp_, e_=e_: e.activation(out=sp_.t[:], in_=e_.t[:], func=AF.Ln, bias=1.0, scale=1.0),
                     r=[e_.b], w=[sp_.b])
                pd = ps[2 + i]
                S.op("pe", lambda e, sp_=sp_, pd=pd: e.matmul(pd.t[:, 0:256], lhsT=trigla_f, rhs=sp_.t[:], start=True, stop=True),
                     r=[sp_.b, cf.b], w=[pd.b])
                S.op("act", lambda e, ed_=ed_, pd=pd: e.activation(out=ed_.t[:], in_=pd.t[:, 0:256], func=AF.Exp), r=[pd.b], w=[ed_.b])
                S.op("dve", lambda e, ed_=ed_, tt=tt: e.tensor_tensor(out=kk.t[:, tt, :], in0=kk.t[:, tt, :], in1=ed_.t[:], op=ALU.mult),
                     r=[ed_.b], w=[kkb[tt]])
                pg = ps[4 + i]

                def mmg(e, sp_=sp_, pg=pg):
                    for h in range(2):
                        ins = e.matmul(pg.t[:, 2 * h:2 * h + 2], lhsT=sp_.t[:, h * 128:(h + 1) * 128], rhs=chind_f, start=True, stop=True)
                    return ins
                S.op("pe", mmg, r=[sp_.b, cf.b], w=[pg.b])
                S.op("act", lambda e, pg=pg, tt=tt: e.activation(out=gch.t[:, tt, :], in_=pg.t[:, 0:4], func=AF.Exp), r=[pg.b], w=[gchb[tt]])
            def kvmm(c, h):
                tt, half = c // 2, c % 2
                pkv = ps[h * 2 + (c % 2)]
                S.op("pe", lambda e: e.matmul(
                    pkv.t[:, 0:256], lhsT=kk.t[64 * half:64 * half + 64, tt, h * 128:(h + 1) * 128],
                    rhs=vv.t[64 * half:64 * half + 64, tt, h * 256:(h + 1) * 256], start=True, stop=True),
                    r=[kkb[tt], vv.b], w=[pkv.b])

            def comb(c, h, otile, po, il, j, cand):
                r0 = 64 * (il % 2)
                fcol = (4 + j) if cand == 0 else j
                if cand == 0:
                    S.op("dve", lambda e: e.tensor_scalar(
                        out=obuf.t[r0:r0 + 64, otile, h * 256:(h + 1) * 256], in0=po.t[r0:r0 + 64, 0:256],
                        scalar1=flg.t[r0:r0 + 64, fcol:fcol + 1], scalar2=None, op0=ALU.mult),
                        r=[po.b, flg.b], w=[obb[otile]])
                else:
                    S.op("dve", lambda e: e.scalar_tensor_tensor(
                        out=obuf.t[r0:r0 + 64, otile, h * 256:(h + 1) * 256], in0=po.t[r0:r0 + 64, 0:256],
                        scalar=flg.t[r0:r0 + 64, fcol:fcol + 1], in1=obuf.t[r0:r0 + 64, otile, h * 256:(h + 1) * 256],
                        op0=ALU.mult, op1=ALU.add), r=[po.b, flg.b], w=[obb[otile]])

            pend = []
            for h in range(2):
                kvmm(0, h)
            for c in range(64):
                tt, half = c // 2, c % 2
                j, cand, il = c // 16, (c % 16) // 8, c % 8
                otile = 4 * j + il // 2
                for h in range(2):
                    pkv = ps[h * 2 + (c % 2)]
                    sf = st_f[h]
                    sb_ = st_b[h][c % 2]
                    if c == 0:
                        S.op("dve", lambda e, sf=sf, pkv=pkv: e.tensor_copy(out=sf.t[:], in_=pkv.t[:, 0:256]), r=[pkv.b], w=[sf.b])
                    else:
                        S.op("dve", lambda e, sf=sf, pkv=pkv, h=h, tt=tt, half=half: e.scalar_tensor_tensor(
                            out=sf.t[:], in0=sf.t[:], scalar=gch.t[:, tt, 2 * h + half:2 * h + half + 1], in1=pkv.t[:, 0:256],
                            op0=ALU.mult, op1=ALU.add), r=[pkv.b, gchb[tt]], w=[sf.b])
                    if c + 1 < 64:
                        kvmm(c + 1, h)
                    S.op("act", lambda e, sf=sf, sb_=sb_: e.activation(out=sb_.t[:], in_=sf.t[:], func=AF.Copy), r=[sf.b], w=[sb_.b])
                    po = ps[4 + h * 2 + (c % 2)]
                    S.op("pe", lambda e, h=h, otile=otile, po=po, sb_=sb_: e.matmul(
                        po.t[:, 0:256], lhsT=qT.t[:, h, otile * 128:(otile + 1) * 128], rhs=sb_.t[:], start=True, stop=True),
                        r=[qT.b, sb_.b], w=[po.b])
                    pend.append((c, h, otile, po, il, j, cand))
                    if len(pend) > 2:
                        comb(*pend.pop(0))
                convert_hook()
            while pend:
                comb(*pend.pop(0))
            for ot in range(16):
                i = ot % 2
                r_, q_, t1_, og_ = rt[i], ssq[i], t1[i], og[i]
                S.dma("sp", lambda e, r_=r_, ot=ot: e.dma_start(out=r_.t[:], in_=s_rg[ot * 128:(ot + 1) * 128, hp * 512:(hp + 1) * 512]), w=[r_.b])
                for h in range(2):
                    S.op("act", lambda e, h=h, ot=ot, q_=q_: e.activation(out=junk.t[:], in_=obuf.t[:, ot, h * 256:(h + 1) * 256], func=AF.Square,
                                                                         accum_out=q_.t[:, h:h + 1]), r=[obb[ot]], w=[junk.b, q_.b])
                S.op("act", lambda e, q_=q_: e.activation(out=q_.t[:, 2:4], in_=q_.t[:, 0:2], func=AF.Ln, bias=EPS, scale=1.0 / 256.0), r=[q_.b], w=[q_.b])
                S.op("act", lambda e, q_=q_: e.activation(out=q_.t[:, 0:2], in_=q_.t[:, 2:4], func=AF.Exp, scale=-0.5), r=[q_.b], w=[q_.b])
                for h in range(2):
                    S.op("dve", lambda e, h=h, ot=ot, q_=q_, t1_=t1_: e.scalar_tensor_tensor(
                        out=t1_.t[:, h * 256:(h + 1) * 256], in0=obuf.t[:, ot, h * 256:(h + 1) * 256], scalar=q_.t[:, h:h + 1],
                        in1=gob.t[:, hp * 512 + h * 256: hp * 512 + (h + 1) * 256], op0=ALU.mult, op1=ALU.mult),
                        r=[obb[ot], q_.b, gob.b], w=[t1_.b])
                S.op("dve", lambda e, t1_=t1_, r_=r_, og_=og_: e.tensor_tensor(out=og_.t[:], in0=t1_.t[:], in1=r_.t[:], op=ALU.mult),
                     r=[t1_.b, r_.b], w=[og_.b])
                pt_ = ps[6 + i]

                def tr(e, og_=og_, pt_=pt_):
                    for cch in range(4):
                        ins = e.transpose(pt_.t[:, cch * 128:(cch + 1) * 128], og_.t[:, cch * 128:(cch + 1) * 128], ident_f)
                    return ins
                S.op("pe", tr, r=[og_.b, cf.b], w=[pt_.b])
                S.op("act", lambda e, pt_=pt_, ot=ot: e.activation(
                    out=ogT.t[:, hp * 4:(hp + 1) * 4, ot * 128:(ot + 1) * 128],
                    in_=pt_.t[:, :].rearrange("p (c t) -> p c t", c=4), func=AF.Copy),
                    r=[pt_.b], w=[ogTb[hp * 4 + cch][ot] for cch in range(4)])
            if debug and hp == 1:
                S.dma("sp", lambda e: e.dma_start(out=dbg["obuf"][:, :], in_=obuf.t[:].rearrange("p a b -> p (a b)")), r=[], w=[])
                S.dma("sp", lambda e: e.dma_start(out=dbg["gch"][:, :], in_=gch.t[:].rearrange("p a b -> p (a b)")), r=[], w=[])
                S.dma("sp", lambda e: e.dma_start(out=dbg["kdec"][:, :], in_=kk.t[:].rearrange("p a b -> p (a b)")), r=[], w=[])
                S.dma("sp", lambda e: e.dma_start(out=dbg["aT"][:, :], in_=s_aT[:, :]), r=[], w=[])
                S.dma("sp", lambda e: e.dma_start(out=dbg["qg"][:, :], in_=s_qg[:, :]), r=[], w=[])
                S.dma("sp", lambda e: e.dma_start(out=dbg["rg"][:, :], in_=s_rg[:, :]), r=[], w=[])
                S.dma("sp", lambda e: e.dma_start(out=dbg["kg"][:, :], in_=s_kg[:, :]), r=[], w=[])
            S.barrier()

        passB(0)
        passB(1)
        S.barrier()

    phaseB()
    A.reset(pmark2)

    def phaseC():
        kT = A.tiles(2, [128, 4096], BF16, "kT")
        vp = A.tiles(2, [128, 32, 128], BF16, "vp")
        qT = A.tiles(2, [128, 2048], BF16, "qTs")
        msk = A.tile([128, 16, 512], BF16, "msk")
        NE, NSP, NEA, NW, NL = 7, 4, 3, 3, 5
        e_t = A.tiles(NE, [128, 1024], BF16, "e")
        sp_t = A.tiles(NSP, [128, 1024], BF16, "sp")
        ea_t = A.tiles(NEA, [128, 1024], BF16, "ea")
        w_t = A.tiles(NW, [128, 1024], BF16, "w")
        la_t = A.tiles(NL, [128, 512], BF16, "la")
        cvfC = A.tiles(4, [128, 1024], F32, "cvfC")
        cvbC = A.tiles(4, [128, 1024], BF16, "cvbC")
        convertC = make_converter(cvfC, cvbC, NCV, pattern=("dve",))
        zp = [(ps[0], ps[1], psall[:, 0:1024]), (ps[2], ps[3], psall[:, 1024:2048])]
        apair = (ps[4], ps[5], psall[:, 2048:3072])
        ob = [ps[6], ps[7]]
        for q in range(4):
            S.dma("sp", lambda e, q=q: e.dma_start(out=msk.t[:, q * 4:(q + 1) * 4, :],
                                                   in_=masks_d[:, q * 2048:(q + 1) * 2048].rearrange("p (m t) -> p m t", m=4)), w=[msk.b])
        pairs = []
        jobn = 0
        for hp in range(8):
            for a in range(2):
                for j in range(4):
                    order = [(kt, True) for kt in range(8 * j + 7, 8 * j - 1, -1)] + [(kt, False) for kt in range(8 * j - 1, -1, -1)]
                    npair = len(order) // 2
                    for pi_ in range(npair):
                        pairs.append(dict(hp=hp, a=a, j=j, kts=(order[2 * pi_], order[2 * pi_ + 1]), first=(pi_ == 0), last=(pi_ == npair - 1),
                                          job=jobn))
                    jobn += 1
        npairs = len(pairs)
        for n, p in enumerate(pairs):
            p["n"] = n
        loaded = {}

        def load_pair(hp):
            i = hp % 2
            S.dma("sp", lambda e: e.dma_start(out=kT[i].t[:], in_=s_ksb[hp * 128:(hp + 1) * 128, :]), w=[kT[i].b])
            S.dma("sp", lambda e: e.dma_start(out=vp[i].t[:], in_=s_vsb[:, hp * 128:(hp + 1) * 128].rearrange("(t p) n -> p t n", p=128)),
                  w=[vp[i].b])
            S.dma("sp", lambda e: e.dma_start(out=qT[i].t[:], in_=s_qsb[hp * 128:(hp + 1) * 128, :]), w=[qT[i].b])
            loaded[hp] = True

        load_pair(0)

        def warm(e):
            for _ in range(64):
                ins = e.matmul(ps[7].t[:, :], lhsT=ones_b, rhs=cb.t[:, 0:512], start=True, stop=True)
            return ins
        S.op("pe", warm, r=[cb.b], w=[ps[7].b])

        def fZ(p):
            n, hp, a, j = p["n"], p["hp"], p["a"], p["j"]
            if p["first"] and a == 0 and j == 0 and hp + 1 < 8 and (hp + 1) not in loaded:
                load_pair(hp + 1)
            pi = hp % 2
            z0, z1, _ = zp[n % 2]

            def f(e):
                for zz, (kt, _) in zip((z0, z1), p["kts"]):
                    ins = e.matmul(zz.t[:, :], lhsT=kT[pi].t[64 * a:64 * a + 64, kt * 128:(kt + 1) * 128],
                                   rhs=qT[pi].t[64 * a:64 * a + 64, j * 512:(j + 1) * 512], start=True, stop=True)
                return ins
            S.op("pe", f, r=[kT[pi].b, qT[pi].b], w=[z0.b, z1.b])

        def fE(p):
            n, j = p["n"], p["j"]
            z0, z1, zap = zp[n % 2]
            e_ = e_t[n % NE]
            S.op("act", lambda e: e.activation(out=e_.t[:, :], in_=zap, func=AF.Exp), r=[z0.b, z1.b], w=[e_.b])
            for h_, (kt, cur) in enumerate(p["kts"]):
                if cur:
                    mi = (j % 2) * 8 + (kt - 8 * j)
                    S.op("dve", lambda e, h_=h_, mi=mi: e.tensor_tensor(out=e_.t[:, h_ * 512:(h_ + 1) * 512], in0=e_.t[:, h_ * 512:(h_ + 1) * 512],
                                                                       in1=msk.t[:, mi, :], op=ALU.mult), r=[msk.b], w=[e_.b])

        def fSP(p):
            n = p["n"]
            e_ = e_t[n % NE]
            sp_ = sp_t[n % NSP]
            S.op("act", lambda e: e.activation(out=sp_.t[:, :], in_=e_.t[:, :], func=AF.Ln, bias=1.0, scale=1.0), r=[e_.b], w=[sp_.b])
            if not p["last"]:
                la_new = la_t[n % NL]
                if p["first"]:
                    S.op("pool", lambda e: e.tensor_tensor(out=la_new.t[:, :], in0=sp_.t[:, 0:512], in1=sp_.t[:, 512:1024], op=ALU.add),
                         r=[sp_.b], w=[la_new.b])
                else:
                    la_old = la_t[(n - 1) % NL]
                    S.op("pool", lambda e: e.tensor_tensor(out=la_new.t[:, :], in0=la_old.t[:, :], in1=sp_.t[:, 0:512], op=ALU.add),
                         r=[sp_.b, la_old.b], w=[la_new.b])
                    S.op("dve", lambda e: e.tensor_tensor(out=la_new.t[:, :], in0=la_new.t[:, :], in1=sp_.t[:, 512:1024], op=ALU.add),
                         r=[sp_.b], w=[la_new.b])

        def fA(p):
            n = p["n"]
            sp_ = sp_t[n % NSP]
            a0, a1, _ = apair
            la_old = None if p["first"] else la_t[(n - 1) % NL]

            def f(e):
                e.matmul(a0.t[:, :], lhsT=trisb_b, rhs=sp_.t[:, 0:512], start=True, stop=(la_old is None))
                if la_old is not None:
                    e.matmul(a0.t[:, :], lhsT=negones_b, rhs=la_old.t[:, :], start=False, stop=True)
                e.matmul(a1.t[:, :], lhsT=trisb_b, rhs=sp_.t[:, 512:1024], start=True, stop=False)
                ins = e.matmul(a1.t[:, :], lhsT=negones_b, rhs=sp_.t[:, 0:512], start=False, stop=(la_old is None))
                if la_old is not None:
                    ins = e.matmul(a1.t[:, :], lhsT=negones_b, rhs=la_old.t[:, :], start=False, stop=True)
                return ins
            S.op("pe", f, r=[sp_.b, cb.b] + ([] if la_old is None else [la_old.b]), w=[a0.b, a1.b])

        def fEA(p):
            n = p["n"]
            a0, a1, aap = apair
            ea_ = ea_t[n % NEA]
            S.op("act", lambda e: e.activation(out=ea_.t[:, :], in_=aap, func=AF.Exp), r=[a0.b, a1.b], w=[ea_.b])

        def fW(p):
            n = p["n"]
            e_, ea_, w_ = e_t[n % NE], ea_t[n % NEA], w_t[n % NW]
            S.op("dve", lambda e: e.tensor_tensor(out=w_.t[:, :], in0=e_.t[:, :], in1=ea_.t[:, :], op=ALU.mult), r=[e_.b, ea_.b], w=[w_.b])

        def fO(p):
            n, hp, a, j = p["n"], p["hp"], p["a"], p["j"]
            pi = hp % 2
            w_ = w_t[n % NW]
            o_ = ob[p["job"] % 2]

            def f(e):
                (kt0, _), (kt1, _) = p["kts"]
                e.matmul(o_.t[:, :], lhsT=vp[pi].t[:, kt0, :], rhs=w_.t[:, 0:512], start=p["first"], stop=False)
                return e.matmul(o_.t[:, :], lhsT=vp[pi].t[:, kt1, :], rhs=w_.t[:, 512:1024], start=False, stop=p["last"])
            S.op("pe", f, r=[vp[pi].b, w_.b], w=[o_.b])
            if p["last"]:
                S.op("act", lambda e: e.activation(out=osbT.t[64 * a:64 * a + 64, hp, j * 512:(j + 1) * 512],
                                                   in_=o_.t[64 * a:64 * a + 64, :], func=AF.Copy), r=[o_.b], w=[osbTb[hp][j]])

        stages = [(fEA, 4), (fZ, 0), (fE, 1), (fSP, 2), (fA, 3), (fW, 5), (fO, 6)]
        for s_ in range(npairs + 7):
            for fn, off in stages:
                if 0 <= s_ - off < npairs:
                    fn(pairs[s_ - off])
            if s_ >= 8 and cvstate["n"] < ((s_ - 8) * NCV) // (npairs - 60) + 1:
                convertC()
        while cvstate["n"] < NCV:
            convertC()
        S.barrier()

    phaseC()
    A.reset(pmark2)

    if debug:
        S.dma("sp", lambda e: e.dma_start(out=dbg["ogT"].rearrange("(c p) t -> p c t", p=128), in_=ogT.t[:]), r=[], w=[])
        S.dma("sp", lambda e: e.dma_start(out=dbg["osbT"].rearrange("(c p) t -> p c t", p=128), in_=osbT.t[:]), r=[], w=[])
        S.barrier()

    hbuf_holder = {}

    def load_w_bf(dst, src_bf, *_):
        S.dma("sp", lambda e: e.dma_start(out=dst.t[:], in_=src_bf.rearrange("(kc p) n -> p kc n", p=128)), w=[dst.b])

    def phaseD():
        hbuf = Tl(nc.alloc_sbuf_tensor_at("hbuf_al", [128, 16, 1024], F32, offset=pmark), "hbuf")
        hb = [Buf() for _ in range(16)]
        hbuf_holder["h"] = (hbuf, hb)
        m = A.mark()
        mixA = A.tile([128, 8, 2048], BF16, "mixA")
        mixb = [Buf() for _ in range(4)]
        Wga = A.tile([128, 8, 1024], BF16, "Wga")
        Wsb = A.tile([128, 8, 1024], BF16, "Wsb")
        stage = A.tiles(2, [128, 8, 128], F32, "wstg")
        sga = A.tiles(2, [128, 512], BF16, "sga")
        sgb = A.tiles(2, [128, 512], BF16, "sgb")
        m1 = A.tiles(2, [128, 512], F32, "m1")
        m2 = A.tiles(2, [128, 512], F32, "m2")
        xt = A.tiles(2, [128, 1024], F32, "xt")
        load_w_bf(Wga, s_wga)
        load_w_bf(Wsb, s_wsb)
        k = 0
        for g in range(4):
            for c in range(8):
                pa, pb_ = ps[(2 * k) % 4], ps[(2 * k + 1) % 4]
                m1_, m2_ = m1[k % 2], m2[k % 2]
                sa_, sb2_ = sga[k % 2], sgb[k % 2]
                k += 1
                S.dma("sp", lambda e, g=g, c=c, sa_=sa_: e.dma_start(out=sa_.t[:], in_=s_sga[c * 128:(c + 1) * 128, g * 512:(g + 1) * 512]),
                      w=[sa_.b])
                S.dma("sp", lambda e, g=g, c=c, sb2_=sb2_: e.dma_start(out=sb2_.t[:], in_=s_sgb[c * 128:(c + 1) * 128, g * 512:(g + 1) * 512]),
                      w=[sb2_.b])

                def mma(e, c=c, g=g, pa=pa):
                    for kc in range(8):
                        ins = e.matmul(pa.t[:, :], lhsT=Wga.t[:, kc, c * 128:(c + 1) * 128], rhs=ogT.t[:, kc, g * 512:(g + 1) * 512],
                                       start=(kc == 0), stop=(kc == 7))
                    return ins

                def mmb(e, c=c, g=g, pb_=pb_):
                    for kc in range(8):
                        ins = e.matmul(pb_.t[:, :], lhsT=Wsb.t[:, kc, c * 128:(c + 1) * 128], rhs=osbT.t[:, kc, g * 512:(g + 1) * 512],
                                       start=(kc == 0), stop=(kc == 7))
                    return ins
                S.op("pe", mma, r=[Wga.b] + [ogTb[kc][4 * g + q] for kc in range(8) for q in range(4)], w=[pa.b])
                S.op("pe", mmb, r=[Wsb.b] + [osbTb[kc][g] for kc in range(8)], w=[pb_.b])
                S.op("dve", lambda e, pa=pa, m1_=m1_, sa_=sa_: e.tensor_tensor(out=m1_.t[:], in0=pa.t[:, :], in1=sa_.t[:], op=ALU.mult),
                     r=[pa.b, sa_.b], w=[m1_.b])
                S.op("dve", lambda e, pb_=pb_, m2_=m2_, sb2_=sb2_: e.tensor_tensor(out=m2_.t[:], in0=pb_.t[:, :], in1=sb2_.t[:], op=ALU.mult),
                     r=[pb_.b, sb2_.b], w=[m2_.b])
                S.op("pool", lambda e, c=c, g=g, m1_=m1_, m2_=m2_: e.tensor_tensor(out=mixA.t[:, c, g * 512:(g + 1) * 512], in0=m1_.t[:],
                                                                                  in1=m2_.t[:], op=ALU.add),
                     r=[m1_.b, m2_.b], w=[mixb[g]])
        S.barrier()
        Wo = Wga
        load_w_bf(Wo, s_wo)
        for g in range(4):
            for tc in range(4):
                ot = 4 * g + tc
                x_ = xt[ot % 2]
                S.dma("sp", lambda e, x_=x_, ot=ot: e.dma_start(out=x_.t[:], in_=x_own[ot * 128:(ot + 1) * 128, :]), w=[x_.b])
                for nh in range(2):
                    ph = ps[4 + (2 * ot + nh) % 4]

                    def mmo(e, ot=ot, nh=nh, ph=ph):
                        for kc in range(8):
                            ins = e.matmul(ph.t[:, :], lhsT=mixA.t[:, kc, ot * 128:(ot + 1) * 128], rhs=Wo.t[:, kc, nh * 512:(nh + 1) * 512],
                                           start=(kc == 0), stop=(kc == 7))
                        return ins
                    S.op("pe", mmo, r=[mixb[g], Wo.b], w=[ph.b])
                    S.op("dve", lambda e, x_=x_, ot=ot, nh=nh, ph=ph: e.tensor_tensor(
                        out=hbuf.t[:, ot, nh * 512:(nh + 1) * 512], in0=ph.t[:, :], in1=x_.t[:, nh * 512:(nh + 1) * 512], op=ALU.add),
                        r=[ph.b, x_.b], w=[hb[ot]])
        S.barrier()
        A.reset(m)

    phaseD()
    hbuf, hb = hbuf_holder["h"]
    if debug:
        S.dma("sp", lambda e: e.dma_start(out=dbg["h1"].rearrange("(t p) n -> p t n", p=128), in_=hbuf.t[:]), r=[], w=[])
        S.barrier()

    eid_all = A.tile([128, 16, 128], I32, "eid_all")
    gate_all = A.tile([128, 16, 128], F32, "gate_all")
    eidb = [Buf() for _ in range(16)]
    gateb = [Buf() for _ in range(16)]
    pmark3 = A.mark()

    def make_rms(junk, sm):
        def rms(src_ap_fn, srcb, gtile, dst, dstb, stat0):
            S.op("act", lambda e: e.activation(out=junk.t[:], in_=src_ap_fn(), func=AF.Square, accum_out=sm.t[:, stat0:stat0 + 1]),
                 r=srcb, w=[junk.b, sm.b])
            S.op("act", lambda e: e.activation(out=sm.t[:, stat0 + 1:stat0 + 2], in_=sm.t[:, stat0:stat0 + 1], func=AF.Ln, bias=EPS,
                                               scale=1.0 / 1024.0), r=[sm.b], w=[sm.b])
            S.op("act", lambda e: e.activation(out=sm.t[:, stat0:stat0 + 1], in_=sm.t[:, stat0 + 1:stat0 + 2], func=AF.Exp, scale=-0.5),
                 r=[sm.b], w=[sm.b])
            S.op("dve", lambda e: e.scalar_tensor_tensor(out=dst.t[:], in0=src_ap_fn(), scalar=sm.t[:, stat0:stat0 + 1], in1=gtile.t[:],
                                                        op0=ALU.mult, op1=ALU.mult), r=srcb + [sm.b, gtile.b], w=dstb)
        return rms

    def transpose_to(dst, dstb, src):
        for hh in range(2):
            pt_ = ps[hh]

            def tr(e, hh=hh, pt_=pt_):
                for c in range(4):
                    ins = e.transpose(pt_.t[:, c * 128:(c + 1) * 128], src.t[:, (hh * 4 + c) * 128:(hh * 4 + c + 1) * 128], ident_f)
                return ins
            S.op("pe", tr, r=[src.b, cf.b], w=[pt_.b])
            S.op("act", lambda e, hh=hh, pt_=pt_: e.activation(out=dst.t[:, hh * 4:(hh + 1) * 4, :],
                                                              in_=pt_.t[:, :].rearrange("p (c t) -> p c t", c=4), func=AF.Copy),
                 r=[pt_.b], w=dstb)

    def phaseE1():
        Wpq = A.tile([128, 8, 2048], BF16, "Wpq")
        k12 = A.tile([128, 2, 1024], BF16, "k12")
        gff = A.tile([128, 1024], F32, "gff")
        m = A.mark()
        stage = A.tiles(2, [128, 8, 128], F32, "wstg")
        load_w_bf(Wpq, s_wpq)
        S.dma("sp", lambda e: e.dma_start(out=k12.t[:, 0, :], in_=s_k1[:, :]), w=[k12.b])
        S.dma("sp", lambda e: e.dma_start(out=k12.t[:, 1, :], in_=s_k2[:, :]), w=[k12.b])
        S.dma("sp", lambda e: e.dma_start(out=gff.t[:], in_=g_ffn_bc[:, :]), w=[gff.b])
        S.barrier()
        A.reset(m)
        xn2 = A.tiles(2, [128, 1024], F32, "xn")
        xnT2 = A.tiles(2, [128, 8, 128], BF16, "xnT")
        qTs2 = A.tiles(2, [128, 16, 128], BF16, "qTs")
        sc2 = A.tiles(2, [128, 16, 128], F32, "sc")
        v12 = A.tile([128, 16, 16], F32, "v12")
        i12 = A.tile([128, 16, 16], U32, "i12")
        i12f = A.tile([128, 16, 16], BF16, "i12h")
        cand = A.tile([128, 8, 256], F32, "cand")
        ts = A.tile([128, 8, 16], F32, "ts")
        eidf = A.tile([128, 128], F32, "eidf")
        junk = A.tile([128, 1024], F32, "junkE")
        posu = A.tile([128, 8, 16], U32, "posu")
        v12b = [Buf() for _ in range(16)]
        i12b = [Buf() for _ in range(16)]
        candb = [Buf() for _ in range(8)]
        tsb = [Buf() for _ in range(8)]
        posb = [Buf() for _ in range(8)]
        abu = A.tile([128, 2, 8, 16], U32, "abu")
        abf = A.tile([128, 2, 8, 16], BF16, "abf")
        oh = A.tile([128, 8, 16, 16], BF16, "oh")
        idab = A.tile([128, 2, 8, 16], F32, "idab")
        sm = A.tile([128, 64], F32, "smg")
        rms_l = [make_rms(junk, A.tile([128, 8], F32, f"smr{i}")) for i in range(2)]

        NCH = 4
        wks = A.tiles(NCH, [128, 256], F32, "wks")

        def top16_multi(chains, n):
            for c0_ in range(0, len(chains), NCH):
                grp = chains[c0_:c0_ + NCH]
                for stage_ in range(5):
                    for ci, (src, srcb, vout, iout, vb_, ib_) in enumerate(grp):
                        wk_ = wks[ci]
                        if stage_ == 0:
                            S.op("dve", lambda e, src=src, vout=vout: e.max(out=vout(0), in_=src()), r=srcb, w=[vb_])
                        elif stage_ == 1:
                            S.op("dve", lambda e, src=src, vout=vout, iout=iout: e.max_index(out=iout(0), in_max=vout(0), in_values=src()),
                                 r=srcb + [vb_], w=[ib_])
                        elif stage_ == 2:
                            S.op("dve", lambda e, src=src, vout=vout, wk_=wk_: e.match_replace(out=wk_.t[:, 0:n], in_to_replace=vout(0),
                                                                                              in_values=src(), imm_value=-1e30),
                                 r=srcb + [vb_], w=[wk_.b])
                        elif stage_ == 3:
                            S.op("dve", lambda e, vout=vout, wk_=wk_: e.max(out=vout(1), in_=wk_.t[:, 0:n]), r=[wk_.b], w=[vb_])
                        else:
                            S.op("dve", lambda e, vout=vout, iout=iout, wk_=wk_: e.max_index(out=iout(1), in_max=vout(1), in_values=wk_.t[:, 0:n]),
                                 r=[wk_.b, vb_], w=[ib_])

        def front(ot):
            hsrc = lambda ot=ot: hbuf.t[:, ot, :]
            xn, xnT, qTs, sc = xn2[ot % 2], xnT2[ot % 2], qTs2[ot % 2], sc2[ot % 2]
            rms_l[ot % 2](hsrc, [hb[ot]], gff, xn, [xn.b], 0)
            transpose_to(xnT, [xnT.b], xn)
            for qb in range(4):
                pq = ps[2 + qb]

                def mmq(e, qb=qb, pq=pq, xnT=xnT):
                    for c in range(4):
                        cc = qb * 4 + c
                        for kc in range(8):
                            ins = e.matmul(pq.t[:, c * 128:(c + 1) * 128], lhsT=Wpq.t[:, kc, cc * 128:(cc + 1) * 128], rhs=xnT.t[:, kc, :],
                                           start=(kc == 0), stop=(kc == 7), skip_group_check=True)
                    return ins
                S.op("pe", mmq, r=[Wpq.b, xnT.b], w=[pq.b])
                S.op("act", lambda e, qb=qb, pq=pq, qTs=qTs: e.activation(out=qTs.t[:, qb * 4:(qb + 1) * 4, :],
                                                                in_=pq.t[:, :].rearrange("p (c t) -> p c t", c=4), func=AF.Copy),
                     r=[pq.b], w=[qTs.b])
            for qb in range(4):
                pq = ps[2 + qb]

                def mms(e, qb=qb, pq=pq, qTs=qTs):
                    for c in range(4):
                        cc = qb * 4 + c
                        hh, half = cc // 2, cc % 2
                        ins = e.matmul(pq.t[:, c * 128:(c + 1) * 128], lhsT=qTs.t[:, cc, :], rhs=k12.t[:, half, hh * 128:(hh + 1) * 128],
                                       start=True, stop=True, skip_group_check=True)
                    return ins
                S.op("pe", mms, r=[qTs.b, k12.b], w=[pq.b])
                S.op("act", lambda e, qb=qb, pq=pq, sc=sc: e.activation(out=sc.t[:, qb * 4:(qb + 1) * 4, :],
                                                                in_=pq.t[:, :].rearrange("p (c t) -> p c t", c=4), func=AF.Copy),
                     r=[pq.b], w=[sc.b])
        def back(ot):
            sc = sc2[ot % 2]
            top16_multi([(lambda cc=cc, sc=sc: sc.t[:, cc, :], [sc.b], lambda r_, cc=cc: v12.t[:, cc, r_ * 8:(r_ + 1) * 8],
                          lambda r_, cc=cc: i12.t[:, cc, r_ * 8:(r_ + 1) * 8], v12b[cc], i12b[cc]) for cc in range(16)], 128)
            S.op("dve", lambda e: e.tensor_copy(out=i12f.t[:], in_=i12.t[:]), r=i12b, w=[i12f.b])
            for hh in range(8):
                S.op("dve", lambda e, hh=hh: e.tensor_tensor(
                    out=cand.t[:, hh, :].rearrange("p (a b) -> p a b", a=16),
                    in0=v12.t[:, 2 * hh, :].unsqueeze(2).to_broadcast([128, 16, 16]),
                    in1=v12.t[:, 2 * hh + 1, :].unsqueeze(1).to_broadcast([128, 16, 16]), op=ALU.add), r=[v12b[2 * hh], v12b[2 * hh + 1]], w=[candb[hh]])
            top16_multi([(lambda hh=hh: cand.t[:, hh, :], [candb[hh]], lambda r_, hh=hh: ts.t[:, hh, r_ * 8:(r_ + 1) * 8],
                          lambda r_, hh=hh: posu.t[:, hh, r_ * 8:(r_ + 1) * 8], tsb[hh], posb[hh]) for hh in range(8)], 256)
            S.op("dve", lambda e: e.tensor_single_scalar(out=abu.t[:, 0], in_=posu.t[:], scalar=4, op=ALU.logical_shift_right), r=posb, w=[abu.b])
            S.op("dve", lambda e: e.tensor_single_scalar(out=abu.t[:, 1], in_=posu.t[:], scalar=15, op=ALU.bitwise_and), r=posb, w=[abu.b])
            S.op("dve", lambda e: e.tensor_copy(out=abf.t[:], in_=abu.t[:]), r=[abu.b], w=[abf.b])
            for half in range(2):
                S.op("dve", lambda e, half=half: e.tensor_tensor(
                    out=oh.t[:], in0=abf.t[:, half].unsqueeze(3).to_broadcast([128, 8, 16, 16]),
                    in1=iota16_bt.t[:, :].unsqueeze(1).unsqueeze(1).to_broadcast([128, 8, 16, 16]), op=ALU.is_equal), r=[abf.b, iota16_bt.b], w=[oh.b])
                S.op("dve", lambda e, half=half: e.tensor_tensor(
                    out=oh.t[:], in0=oh.t[:],
                    in1=i12f.t[:].rearrange("p (h a) n -> p h a n", a=2)[:, :, half, :].unsqueeze(2).to_broadcast([128, 8, 16, 16]),
                    op=ALU.mult), r=[i12f.b], w=[oh.b])
                S.op("dve", lambda e, half=half: e.tensor_reduce(out=idab.t[:, half], in_=oh.t[:], axis=mybir.AxisListType.X, op=ALU.add),
                     r=[oh.b], w=[idab.b])
            S.op("dve", lambda e: e.scalar_tensor_tensor(out=eidf.t[:].rearrange("p (h k) -> p h k", h=8), in0=idab.t[:, 0], scalar=128.0,
                                                        in1=idab.t[:, 1], op0=ALU.mult, op1=ALU.add), r=[idab.b], w=[eidf.b])
            S.op("dve", lambda e, ot=ot: e.tensor_copy(out=eid_all.t[:, ot, :], in_=eidf.t[:]), r=[eidf.b], w=[eidb[ot]])
            if debug:
                S.dma("sp", lambda e, ot=ot: e.dma_start(out=dbg["eid"][ot * 128:(ot + 1) * 128, :], in_=eid_all.t[:, ot, :]), r=[eidb[ot]], w=[])
            for hh in range(8):
                S.op("dve", lambda e, hh=hh: e.tensor_scalar(out=sm.t[:, 8 + hh:9 + hh], in0=ts.t[:, hh, 0:1], scalar1=-1.0, scalar2=None,
                                                             op0=ALU.mult), r=[tsb[hh]], w=[sm.b])

        def gates(ot):
            for hh in range(8):
                S.op("act", lambda e, hh=hh, ot=ot: e.activation(out=gate_all.t[:, ot, hh * 16:(hh + 1) * 16], in_=ts.t[:, hh, :], func=AF.Exp,
                                                                 bias=sm.t[:, 8 + hh:9 + hh], scale=1.0, accum_out=sm.t[:, 16 + hh:17 + hh]),
                     r=[tsb[hh], sm.b], w=[gateb[ot], sm.b])
            S.op("dve", lambda e: e.reciprocal(out=sm.t[:, 24:32], in_=sm.t[:, 16:24]), r=[sm.b], w=[sm.b])
            S.op("dve", lambda e, ot=ot: e.tensor_tensor(
                out=gate_all.t[:, ot, :].rearrange("p (h k) -> p h k", h=8), in0=gate_all.t[:, ot, :].rearrange("p (h k) -> p h k", h=8),
                in1=sm.t[:, 24:32].unsqueeze(2).to_broadcast([128, 8, 16]), op=ALU.mult), r=[sm.b], w=[gateb[ot]])
        front(0)
        for ot in range(16):
            if ot >= 1:
                gates(ot - 1)
            if ot + 1 < 16:
                front(ot + 1)
            back(ot)
        gates(15)
        S.barrier()

    phaseE1()
    A.reset(pmark3)

    def phaseE2():
        GK = 4
        NB = 26
        gff = A.tile([128, 1024], F32, "gff2")
        xn = A.tile([128, 1024], BF16, "xn2")
        junk = A.tile([128, 1024], F32, "junkE2")
        junkb = A.tile([128, 1024], BF16, "junkE2b")
        sm = A.tile([128, 64], F32, "sm2")
        hd = A.tile([128, 128], F32, "hd")
        ga_ = A.tile([128, 128], F32, "ga")
        tq = A.tiles(4, [128, 128], F32, "tq")
        uv = A.tiles(NB, [128, 2048], BF16, "uv")
        dg = A.tiles(6, [128, 128], BF16, "dg")
        S.dma("sp", lambda e: e.dma_start(out=gff.t[:], in_=g_ffn_bc[:, :]), w=[gff.b])
        rms = make_rms(junk, sm)
        NG = 128 // GK
        hdb = [Buf() for _ in range(NG)]
        gab = [Buf() for _ in range(NG)]
        tqb = [[Buf() for _ in range(NG)] for _ in range(4)]
        cnt = {"u": 0, "d": 0}
        t0, t1_, t2, t3 = tq
        for ot in range(16):
            hsrc = lambda ot=ot: hbuf.t[:, ot, :]
            rms(hsrc, [hb[ot]], gff, xn, [xn.b], 0)
            held = {}
            pacc = (ps[4 + 2 * (ot % 2)], ps[5 + 2 * (ot % 2)])

            def post(g, ot=ot, pacc=pacc):
                sl = slice(g * GK, (g + 1) * GK)
                S.op("dve", lambda e: e.tensor_tensor(out=t0.t[:, sl], in0=t3.t[:, sl], in1=hd.t[:, sl], op=ALU.mult),
                     r=[tqb[3][g], hdb[g]], w=[tqb[0][g]])
                S.op("dve", lambda e: e.tensor_tensor(out=ga_.t[:, sl], in0=t0.t[:, sl], in1=gate_all.t[:, ot, sl], op=ALU.mult),
                     r=[tqb[0][g], gateb[ot]], w=[gab[g]])
                for k in range(GK):
                    kk_ = g * GK + k
                    b_ = held[kk_]
                    d_ = dg[cnt["d"] % 6]
                    cnt["d"] += 1
                    S.op("act", lambda e, d_=d_, kk_=kk_: e.activation(out=d_.t[:], in_=ident_f, func=AF.Copy, scale=ga_.t[:, kk_:kk_ + 1]),
                         r=[gab[g], cf.b], w=[d_.b])

                    def mmv(e, d_=d_, b_=b_, kk_=kk_):
                        e.matmul(pacc[0].t[:, :], lhsT=d_.t[:], rhs=b_.t[:, 1024:1536], start=(kk_ == 0), stop=(kk_ == 127))
                        return e.matmul(pacc[1].t[:, :], lhsT=d_.t[:], rhs=b_.t[:, 1536:2048], start=(kk_ == 0), stop=(kk_ == 127))
                    S.op("pe", mmv, r=[d_.b, b_.b], w=[pacc[0].b, pacc[1].b])

            for g in range(NG):
                sl = slice(g * GK, (g + 1) * GK)
                for k in range(GK):
                    kk_ = g * GK + k
                    b_ = uv[cnt["u"] % NB]
                    cnt["u"] += 1
                    held[kk_] = b_
                    S.dma("pool", lambda e, b_=b_, kk_=kk_, ot=ot: e.indirect_dma_start(
                        out=b_.t[:, :], out_offset=None, in_=s_uvb[:, :],
                        in_offset=bass.IndirectOffsetOnAxis(ap=eid_all.t[:, ot, kk_:kk_ + 1], axis=0)), r=[eidb[ot]], w=[b_.b])
                    S.op("dve", lambda e, b_=b_, kk_=kk_: e.scalar_tensor_tensor(
                        out=junkb.t[:], in0=b_.t[:, 0:1024], scalar=1.0, in1=xn.t[:], op0=ALU.mult, op1=ALU.mult,
                        accum_out=hd.t[:, kk_:kk_ + 1]), r=[b_.b, xn.b], w=[junkb.b, hdb[g]])
                S.op("act", lambda e, sl=sl: e.activation(out=t1_.t[:, sl], in_=hd.t[:, sl], func=AF.Square, scale=0.21145921592590275),
                     r=[hdb[g]], w=[tqb[1][g]])
                if g >= 1:
                    post(g - 1)
                S.op("dve", lambda e, sl=sl: e.scalar_tensor_tensor(out=t2.t[:, sl], in0=t1_.t[:, sl], scalar=1.0, in1=hd.t[:, sl],
                                                                    op0=ALU.add, op1=ALU.mult),
                     r=[tqb[1][g], hdb[g]], w=[tqb[2][g]])
                S.op("act", lambda e, sl=sl: e.activation(out=t3.t[:, sl], in_=t2.t[:, sl], func=AF.Sigmoid, scale=1.5957691216057308),
                     r=[tqb[2][g]], w=[tqb[3][g]])
            post(NG - 1)
            for nh in range(2):
                S.op("dve", lambda e, ot=ot, nh=nh, pacc=pacc: e.tensor_tensor(
                    out=hbuf.t[:, ot, nh * 512:(nh + 1) * 512], in0=pacc[nh].t[:, :], in1=hbuf.t[:, ot, nh * 512:(nh + 1) * 512], op=ALU.add),
                    r=[pacc[nh].b], w=[hb[ot]])
            if debug:
                S.dma("sp", lambda e, ot=ot: e.dma_start(out=dbg["h2"][ot * 128:(ot + 1) * 128, :], in_=hbuf.t[:, ot, :]), r=[hb[ot]], w=[])
        S.barrier()

    phaseE2()
    A.reset(pmark2)

    def phaseF():
        Wpg = A.tile([128, 8, 1024], BF16, "Wpg")
        Wpl = A.tile([128, 2, 1024], BF16, "Wpl")
        pTb = A.tile([128, 2, 2048], BF16, "pTb")
        gpl = A.tile([128, 1024], F32, "gpl")
        gfi = A.tile([128, 1024], F32, "gfi")
        stage = A.tiles(2, [128, 8, 128], F32, "wstg")
        stage2 = A.tiles(2, [128, 2, 512], F32, "wstg2")
        load_w_bf(Wpg, s_wpg)
        load_w_bf(Wpl, s_wpl)
        S.dma("sp", lambda e: e.dma_start(out=pTb.t[:], in_=s_pT.rearrange("(kc p) n -> p kc n", p=128)), w=[pTb.b])
        S.dma("sp", lambda e: e.dma_start(out=gpl.t[:], in_=g_ple_bc[:, :]), w=[gpl.b])
        S.dma("sp", lambda e: e.dma_start(out=gfi.t[:], in_=g_fin_bc[:, :]), w=[gfi.b])
        xn = A.tiles(2, [128, 1024], F32, "xnF")
        xnT = A.tiles(2, [128, 8, 128], BF16, "xnTF")
        junk = A.tile([128, 1024], F32, "junkF")
        sig = A.tiles(2, [128, 1024], F32, "sigF")
        tmp = A.tiles(2, [128, 1024], F32, "tmpF")
        outt = A.tiles(2, [128, 1024], F32, "outF")
        sm = A.tile([128, 64], F32, "smF")
        rms1 = make_rms(junk, sm)
        rms2 = make_rms(A.tile([128, 1024], F32, "junkF2"), A.tile([128, 8], F32, "smF2"))

        def front(ot):
            i = ot % 2
            hsrc = lambda ot=ot: hbuf.t[:, ot, :]
            rms1(hsrc, [hb[ot]], gpl, xn[i], [xn[i].b], 0)
            transpose_to(xnT[i], [xnT[i].b], xn[i])
            for nh in range(2):
                pg_, pp_ = ps[2 + nh], ps[4 + 2 * i + nh]

                def mmg(e, nh=nh, pg_=pg_, i=i):
                    for kc in range(8):
                        ins = e.matmul(pg_.t[:, :], lhsT=xnT[i].t[:, kc, :], rhs=Wpg.t[:, kc, nh * 512:(nh + 1) * 512], start=(kc == 0), stop=(kc == 7))
                    return ins

                def mmp(e, nh=nh, pp_=pp_, ot=ot):
                    for kc in range(2):
                        ins = e.matmul(pp_.t[:, :], lhsT=pTb.t[:, kc, ot * 128:(ot + 1) * 128], rhs=Wpl.t[:, kc, nh * 512:(nh + 1) * 512],
                                       start=(kc == 0), stop=(kc == 1))
                    return ins
                S.op("pe", mmg, r=[xnT[i].b, Wpg.b], w=[pg_.b])
                S.op("pe", mmp, r=[pTb.b, Wpl.b], w=[pp_.b])
                S.op("act", lambda e, nh=nh, pg_=pg_, i=i: e.activation(out=sig[i].t[:, nh * 512:(nh + 1) * 512], in_=pg_.t[:, :], func=AF.Sigmoid),
                     r=[pg_.b], w=[sig[i].b])

        def back(ot):
            i = ot % 2
            hsrc = lambda ot=ot: hbuf.t[:, ot, :]
            for nh in range(2):
                pp_ = ps[4 + 2 * i + nh]
                S.op("dve", lambda e, nh=nh, pp_=pp_, i=i: e.tensor_tensor(out=tmp[i].t[:, nh * 512:(nh + 1) * 512], in0=pp_.t[:, :],
                                                                          in1=sig[i].t[:, nh * 512:(nh + 1) * 512], op=ALU.mult),
                     r=[pp_.b, sig[i].b], w=[tmp[i].b])
            S.op("dve", lambda e, ot=ot, i=i: e.tensor_tensor(out=hbuf.t[:, ot, :], in0=hbuf.t[:, ot, :], in1=tmp[i].t[:], op=ALU.add),
                 r=[tmp[i].b], w=[hb[ot]])
            o_ = outt[i]
            rms2(hsrc, [hb[ot]], gfi, o_, [o_.b], 0)
            S.dma("sp", lambda e, ot=ot, o_=o_: e.dma_start(out=out_d[ot * 128:(ot + 1) * 128, :], in_=o_.t[:]), r=[o_.b], w=[])

        front(0)
        for ot in range(16):
            if ot + 1 < 16:
                front(ot + 1)
            back(ot)

    phaseF()
    S.barrier()
    S.emit()
    return nc


def _consts():
    c = np.zeros((128, 6 * 128 + 2 + 16), np.float32)
    j = np.arange(128)[:, None]
    s = np.arange(128)[None, :]
    c[:, 0:128] = np.eye(128, dtype=np.float32)
    c[:, 128:256] = 1.0
    c[:, 256:384] = np.where(j >= s, -1.0, 0.0)
    c[:, 384:512] = -1.0
    c[:, 512:640] = np.where((j > s) & ((j // 64) == (s // 64)), -1.0 / 16.0, 0.0)
    c[:, 640:768] = np.where(j < s, 1.0, 0.0)
    c[:, 768] = np.where(np.arange(128) < 64, -1.0 / 16.0, 0.0)
    c[:, 769] = np.where(np.arange(128) >= 64, -1.0 / 16.0, 0.0)
    c[:, 770:786] = np.arange(16, dtype=np.float32)[None, :]
    return c


def _masks(hf):
    sp = np.arange(128)[:, None]
    t = np.arange(512)[None, :]
    qb, tq = t // 128, t % 128
    m = np.zeros((128, 2, 8, 512), np.float32)
    for par in range(2):
        delta = (par + hf) % 2
        for i in range(4):
            bt = ((qb > i) | ((qb == i) & (sp < tq))).astype(np.float32)
            if delta == 0:
                m[:, par, i] = bt
                m[:, par, 4 + i] = 0.0
            else:
                m[:, par, i] = 1.0
                m[:, par, 4 + i] = bt
    return np.ascontiguousarray(m.reshape(128, 16 * 512)).astype(ml_dtypes.bfloat16)


def _own_groups(hf):
    delta = [(j + hf) % 2 for j in range(4)]
    return [2 * j + delta[j] for j in range(4)], delta


def _core_inputs(c, inp, shared):
    b, hf = c // 2, c % 2
    own, delta = _own_groups(hf)
    x = inp["x"][b]
    xT = np.ascontiguousarray(x.T)
    xg = x.reshape(8, 512, 1024)
    x_own = np.ascontiguousarray(xg[own].reshape(2048, 1024))
    p_own = inp["p"][0, b].reshape(8, 512, 256)[own].reshape(2048, 256)
    flags = np.zeros((128, 8), np.float32)
    for j in range(4):
        flags[:, j] = float(delta[j])
        flags[:, 4 + j] = 1.0 - float(delta[j])
    d = dict(shared)
    d.update({
        "xT_true": xT,
        "xT_own": np.ascontiguousarray(x_own.T),
        "masks": _masks(hf),
        "x_own": x_own,
        "pT_own": np.ascontiguousarray(p_own.T),
        "flags": flags,
    })
    return d


def _shared_inputs(inp):
    f = np.float32
    bc = lambda v: np.ascontiguousarray(np.broadcast_to(np.asarray(v, f).reshape(1, -1), (128, v.size)))
    return {
        "w_in": np.ascontiguousarray(inp["w_in"][0], f),
        "w_a_up": np.ascontiguousarray(inp["w_gla_a_up"][0], f),
        "b_a_bc": bc(inp["b_gla_a"][0]),
        "g_mix_pk": np.ascontiguousarray(np.asarray(inp["g_mix"][0], f).reshape(8, 128).T),
        "g_o_bc": bc(inp["g_gla_o"][0]),
        "w_gla_out": np.ascontiguousarray(inp["w_gla_out"][0], f),
        "w_sb_out": np.ascontiguousarray(inp["w_sb_out"][0], f),
        "w_o": np.ascontiguousarray(inp["w_o"][0], f),
        "g_ffn_bc": bc(inp["g_ffn"][0]),
        "w_pq": np.ascontiguousarray(inp["w_peer_q"][0], f),
        "k1T": np.ascontiguousarray(np.asarray(inp["peer_k1"][0], f).transpose(2, 0, 1).reshape(128, 1024)),
        "k2T": np.ascontiguousarray(np.asarray(inp["peer_k2"][0], f).transpose(2, 0, 1).reshape(128, 1024)),
        "peer_uv": np.ascontiguousarray(np.concatenate([np.asarray(inp["peer_u"][0], f), np.asarray(inp["peer_v"][0], f)], axis=1)),
        "g_ple_bc": bc(inp["g_ple"][0]),
        "w_pg": np.ascontiguousarray(inp["w_ple_gate"][0], f),
        "w_ple": np.ascontiguousarray(inp["w_ple"][0], f),
        "g_fin_bc": bc(inp["g_final"]),
        "consts": _consts(),
    }


def kernel(**inputs):
    inp = {k: np.asarray(v) for k, v in inputs.items()}
    shared = _shared_inputs(inp)
    in_maps = [_core_inputs(c, inp, shared) for c in range(8)]
    nc = build()
    res = run_bass_kernel_spmd(nc, in_maps, core_ids=list(range(8)))
    out = np.zeros((4, 8, 512, 1024), np.float32)
    for c in range(8):
        b, hf = c // 2, c % 2
        own, _ = _own_groups(hf)
        o = np.asarray(res.results[c]["out"]).reshape(4, 512, 1024)
        for j in range(4):
            out[b, own[j]] = o[j]
    return out.reshape(4, 4096, 1024)
```

```python
import numpy as np
import ml_dtypes
import concourse.bass as bass
import concourse.mybir as mybir
from concourse.bass_utils import run_bass_kernel_spmd

F32 = mybir.dt.float32
BF16 = mybir.dt.bfloat16
I32 = mybir.dt.int32
U32 = mybir.dt.uint32
AF = mybir.ActivationFunctionType
ALU = mybir.AluOpType

EPS = 1e-6
SB_BASE = 16512
SB_LIMIT = 229344


class Buf:
    __slots__ = ("w", "rs", "name")

    def __init__(self, name=""):
        self.w = None
        self.rs = []
        self.name = name


class Sched:
    EPOCH = 4096
    NDMA = {"sp": 24, "pool": 24, "act": 8}

    def __init__(self, nc):
        self.nc = nc
        self.names = ["pe", "act", "dve", "pool", "sp"]
        self.ops = {n: [] for n in self.names}
        self.count = {n: 0 for n in self.names}
        self.esems = {n: [] for n in self.names}
        self.waited = {n: {} for n in self.names}
        self.dsems = {q: [] for q in self.NDMA}
        self.duse = {q: [] for q in self.NDMA}
        self.dnext = {q: 0 for q in self.NDMA}
        self.semctx = []

    def _newsem(self, name):
        cm = self.nc.semaphore(name)
        s = cm.__enter__()
        self.semctx.append(cm)
        return s

    def _tok_compute(self, eng):
        n = self.count[eng]
        ep, idx = divmod(n, self.EPOCH)
        while len(self.esems[eng]) <= ep:
            self.esems[eng].append(self._newsem(f"e_{eng}_{len(self.esems[eng])}"))
        self.count[eng] = n + 1
        return (self.esems[eng][ep], idx + 1)

    def _deps(self, r, w):
        deps = []
        for b in r:
            if b.w is not None:
                deps.append(b.w)
        for b in w:
            if b.w is not None:
                deps.append(b.w)
            deps.extend(b.rs)
        return deps

    def _commit(self, tok, r, w):
        for b in r:
            b.rs.append(tok)
        for b in w:
            b.w = tok
            b.rs = []

    def _waits(self, eng, deps):
        best = {}
        for (s, v) in deps:
            k = id(s)
            if k not in best or best[k][1] < v:
                best[k] = (s, v)
        out = []
        wd = self.waited[eng]
        for k, (s, v) in best.items():
            if wd.get(k, 0) >= v:
                continue
            wd[k] = v
            out.append((s, v))
        return out

    def op(self, eng, fn, r=(), w=()):
        waits = self._waits(eng, self._deps(r, w))
        tok = self._tok_compute(eng)
        self.ops[eng].append((waits, fn, (tok[0], 1)))
        self._commit(tok, r, w)
        return tok

    def dma(self, q, fn, r=(), w=()):
        if len(self.dsems[q]) < self.NDMA[q]:
            self.dsems[q].append(self._newsem(f"d_{q}_{len(self.dsems[q])}"))
            self.duse[q].append(0)
        i = self.dnext[q]
        self.dnext[q] = (i + 1) % self.NDMA[q]
        s = self.dsems[q][i]
        deps = self._deps(r, w)
        if self.duse[q][i] > 0:
            deps.append((s, 16 * self.duse[q][i]))
        waits = self._waits(q, deps)
        self.duse[q][i] += 1
        tok = (s, 16 * self.duse[q][i])
        self.ops[q].append((waits, fn, (s, 16)))
        self._commit(tok, r, w)
        return tok

    def _all_tokens(self):
        deps = []
        for n in self.names:
            c = self.count[n]
            if c > 0:
                ep, idx = divmod(c - 1, self.EPOCH)
                deps.append((self.esems[n][ep], idx + 1))
        for q in self.NDMA:
            for s, u in zip(self.dsems[q], self.duse[q]):
                if u > 0:
                    deps.append((s, 16 * u))
        return deps

    def barrier(self, engines=None):
        deps = self._all_tokens()
        for n in (engines or self.names):
            waits = self._waits(n, list(deps))
            if waits:
                self.ops[n].append((waits, None, None))

    def emit(self):
        nc = self.nc
        ops = self.ops

        def replay(e, lst):
            for waits, fn, inc in lst:
                for (s, v) in waits:
                    e.wait_ge(s, v)
                if fn is None:
                    continue
                ins = fn(e)
                ins.then_inc(inc[0], inc[1])

        with nc.Block() as block:
            @block.tensor
            def _(e):
                replay(e, ops["pe"])

            @block.scalar
            def _(e):
                replay(e, ops["act"])

            @block.vector
            def _(e):
                replay(e, ops["dve"])

            @block.gpsimd
            def _(e):
                replay(e, ops["pool"])

            @block.sync
            def _(e):
                replay(e, ops["sp"])


class Tl:
    __slots__ = ("t", "b")

    def __init__(self, t, name=""):
        self.t = t
        self.b = Buf(name)


class Alloc:
    def __init__(self, nc):
        self.nc = nc
        self.off = SB_BASE
        self.n = 0

    def mark(self):
        return self.off

    def reset(self, m):
        self.off = m

    def tile(self, shape, dt, name="t"):
        esz = {F32: 4, BF16: 2, I32: 4, U32: 4}[dt]
        nbytes = int(np.prod(shape[1:])) * esz
        nbytes = (nbytes + 31) // 32 * 32
        assert self.off + nbytes <= SB_LIMIT, f"SBUF overflow at {name}: {self.off}+{nbytes}"
        self.n += 1
        t = self.nc.alloc_sbuf_tensor_at(f"{name}_{self.n}", list(shape), dt, offset=self.off)
        self.off += nbytes
        return Tl(t, name)

    def tiles(self, k, shape, dt, name="t"):
        return [self.tile(shape, dt, f"{name}{i}") for i in range(k)]


C_GQ, C_GK, C_GV, C_GR, C_GA, C_SQ, C_SK, C_SV, C_GTA, C_GTB = 0, 512, 1024, 2048, 3072, 3088, 4112, 5136, 6160, 7184


def build(debug=False):
    nc = bass.Bass("TRN2", target_bir_lowering=False)

    def din(name, shape, dt=F32):
        return nc.dram_tensor(name, list(shape), dt, kind="ExternalInput").ap()

    def dscr(name, shape, dt=BF16):
        return nc.dram_tensor(name, list(shape), dt, kind="Internal").ap()

    xT_true = din("xT_true", [1024, 4096])
    xT_own = din("xT_own", [1024, 2048])
    masks_d = din("masks", [128, 16 * 512], BF16)
    x_own = din("x_own", [2048, 1024])
    pT_own = din("pT_own", [256, 2048])
    flags_d = din("flags", [128, 8])
    w_in = din("w_in", [1024, 8208])
    w_a_up = din("w_a_up", [16, 512])
    b_a_bc = din("b_a_bc", [128, 512])
    g_mix_pk = din("g_mix_pk", [128, 8])
    g_o_bc = din("g_o_bc", [128, 1024])
    w_gla_out = din("w_gla_out", [1024, 1024])
    w_sb_out = din("w_sb_out", [1024, 1024])
    w_o = din("w_o", [1024, 1024])
    g_ffn_bc = din("g_ffn_bc", [128, 1024])
    w_pq = din("w_pq", [1024, 2048])
    k1T = din("k1T", [128, 1024])
    k2T = din("k2T", [128, 1024])
    peer_uv = din("peer_uv", [16384, 2048])
    g_ple_bc = din("g_ple_bc", [128, 1024])
    w_pg = din("w_pg", [1024, 1024])
    w_ple = din("w_ple", [256, 1024])
    g_fin_bc = din("g_fin_bc", [128, 1024])
    consts_d = din("consts", [128, 6 * 128 + 2 + 16])
    out_d = nc.dram_tensor("out", [2048, 1024], F32, kind="ExternalOutput").ap()

    s_kg = dscr("s_kg", [4096, 512])
    s_vg = dscr("s_vg", [4096, 1024])
    s_aT = dscr("s_aT", [16, 4096], F32)
    s_qg = dscr("s_qg", [512, 2048])
    s_rg = dscr("s_rg", [2048, 1024])
    s_ksb = dscr("s_ksb", [1024, 4096])
    s_vsb = dscr("s_vsb", [4096, 1024])
    s_qsb = dscr("s_qsb", [1024, 2048])
    s_sga = dscr("s_sga", [1024, 2048])
    s_sgb = dscr("s_sgb", [1024, 2048])
    s_wga = dscr("s_wga", [1024, 1024])
    s_wsb = dscr("s_wsb", [1024, 1024])
    s_wo = dscr("s_wo", [1024, 1024])
    s_wpg = dscr("s_wpg", [1024, 1024])
    s_wpq = dscr("s_wpq", [1024, 2048])
    s_wpl = dscr("s_wpl", [256, 1024])
    s_k1 = dscr("s_k1", [128, 1024])
    s_k2 = dscr("s_k2", [128, 1024])
    s_pT = dscr("s_pT", [256, 2048])
    s_uvb = dscr("s_uvb", [16384, 2048])

    dbg = {}
    if debug:
        dbg["h1"] = nc.dram_tensor("dbg_h1", [2048, 1024], F32, kind="ExternalOutput").ap()
        dbg["ogT"] = nc.dram_tensor("dbg_ogT", [1024, 2048], BF16, kind="ExternalOutput").ap()
        dbg["osbT"] = nc.dram_tensor("dbg_osbT", [1024, 2048], BF16, kind="ExternalOutput").ap()
        dbg["h2"] = nc.dram_tensor("dbg_h2", [2048, 1024], F32, kind="ExternalOutput").ap()
        dbg["eid"] = nc.dram_tensor("dbg_eid", [2048, 128], I32, kind="ExternalOutput").ap()
        dbg["aT"] = nc.dram_tensor("dbg_aT", [16, 4096], F32, kind="ExternalOutput").ap()
        dbg["qg"] = nc.dram_tensor("dbg_qg", [512, 2048], BF16, kind="ExternalOutput").ap()
        dbg["rg"] = nc.dram_tensor("dbg_rg", [2048, 1024], BF16, kind="ExternalOutput").ap()
        dbg["kg"] = nc.dram_tensor("dbg_kg", [4096, 512], BF16, kind="ExternalOutput").ap()
        dbg["obuf"] = nc.dram_tensor("dbg_obuf", [128, 16 * 512], F32, kind="ExternalOutput").ap()
        dbg["gch"] = nc.dram_tensor("dbg_gch", [128, 128], F32, kind="ExternalOutput").ap()
        dbg["kdec"] = nc.dram_tensor("dbg_kdec", [128, 32 * 256], BF16, kind="ExternalOutput").ap()

    S = Sched(nc)
    A = Alloc(nc)
    psall = nc.alloc_psum_tensor("psall", [128, 4096], F32)
    ps = [Tl(psall[:, i * 512:(i + 1) * 512], f"ps{i}") for i in range(8)]

    cf = A.tile([128, 6 * 128 + 2 + 16], F32, "cf")
    cb = A.tile([128, 5 * 128], BF16, "cb")
    flg = A.tile([128, 8], F32, "flg")
    S.dma("sp", lambda e: e.dma_start(out=cf.t[:], in_=consts_d[:, :]), w=[cf.b])
    S.dma("sp", lambda e: e.dma_start(out=flg.t[:], in_=flags_d[:, :]), w=[flg.b])
    S.op("act", lambda e: e.activation(out=cb.t[:], in_=cf.t[:, 128:768], func=AF.Copy), r=[cf.b], w=[cb.b])
    ident_f = cf.t[:, 0:128]
    ones_b = cb.t[:, 0:128]
    trisb_b = cb.t[:, 128:256]
    negones_b = cb.t[:, 256:384]
    trigla_f = cf.t[:, 512:640]
    trimask_f = cf.t[:, 640:768]
    chind_f = cf.t[:, 768:770]
    iota16_f = cf.t[:, 770:786]
    iota16_bt = A.tile([128, 16], BF16, "iota16b")
    S.op("act", lambda e: e.activation(out=iota16_bt.t[:], in_=iota16_f, func=AF.Copy), r=[cf.b], w=[iota16_bt.b])
    pmark = A.mark()

    cvstate = {"n": 0}
    uv_src = peer_uv.rearrange("(p r) n -> p r n", p=128)
    uv_dst = s_uvb.rearrange("(p r) n -> p r n", p=128)
    cvitems = []
    for (wsrc, wdst, rows, cols) in ((w_gla_out, s_wga, 1024, 1024), (w_sb_out, s_wsb, 1024, 1024), (w_o, s_wo, 1024, 1024),
                                     (w_pq, s_wpq, 1024, 2048), (k1T, s_k1, 128, 1024), (k2T, s_k2, 128, 1024),
                                     (w_pg, s_wpg, 1024, 1024), (w_ple, s_wpl, 256, 1024), (pT_own, s_pT, 256, 2048)):
        for r0 in range(0, rows, 128):
            for c0_ in range(0, cols, 1024):
                cvitems.append((wsrc[r0:r0 + 128, c0_:c0_ + 1024], wdst[r0:r0 + 128, c0_:c0_ + 1024]))
    for c in range(256):
        r_, hcol = c // 2, (c % 2) * 1024
        cvitems.append((uv_src[:, r_, hcol:hcol + 1024], uv_dst[:, r_, hcol:hcol + 1024]))
    NCV = len(cvitems)

    def make_converter(cvf, cvb, limit, pattern=("act", "dve")):
        nb_ = len(cvf)

        def convert_chunk():
            c = cvstate["n"]
            if c >= limit:
                return
            cvstate["n"] += 1
            src_, dst_ = cvitems[c]
            f_, b_ = cvf[c % nb_], cvb[c % nb_]
            S.dma("sp", lambda e: e.dma_start(out=f_.t[:], in_=src_), w=[f_.b])
            if pattern[c % len(pattern)] == "act":
                S.op("act", lambda e: e.activation(out=b_.t[:], in_=f_.t[:], func=AF.Copy), r=[f_.b], w=[b_.b])
            else:
                S.op("dve", lambda e: e.tensor_copy(out=b_.t[:], in_=f_.t[:]), r=[f_.b], w=[b_.b])
            S.dma("pool", lambda e: e.dma_start(out=dst_, in_=b_.t[:]), r=[b_.b], w=[])
        return convert_chunk

    def phaseA():
        uT = A.tile([128, 8, 4096], BF16, "uT")
        uTb = [Buf(f"uT{g}") for g in range(8)]
        xs = A.tiles(2, [128, 8, 512], F32, "xs")
        sq = A.tiles(2, [128, 8, 512], BF16, "sq")
        lnv = A.tiles(2, [128, 512], F32, "lnv")
        rstd = A.tiles(2, [128, 512], F32, "rstd")
        Wf = A.tiles(2, [128, 8, 512], F32, "Wf")
        Wb = A.tiles(2, [128, 8, 512], BF16, "Wb")
        ostg = A.tiles(4, [128, 512], BF16, "ostg")
        ostg_f = A.tile([16, 512], F32, "ostgf")
        gmix = A.tile([128, 8], F32, "gmix")
        S.dma("sp", lambda e: e.dma_start(out=gmix.t[:], in_=g_mix_pk[:, :]), w=[gmix.b])
        cnt = {"n": 0, "w": 0, "o": 0, "ps": 0}
        convert_chunk = make_converter([None], [None], 0)

        def norm(xT, ngroups):
            for g in range(ngroups):
                i = cnt["n"] % 2
                cnt["n"] += 1
                x_, sq_, ln_, rs_ = xs[i], sq[i], lnv[i], rstd[i]
                S.dma("sp", lambda e, x_=x_, g=g: e.dma_start(
                    out=x_.t[:], in_=xT[:, g * 512:(g + 1) * 512].rearrange("(kc p) t -> p kc t", p=128)), w=[x_.b])
                S.op("act", lambda e, x_=x_, sq_=sq_: e.activation(out=sq_.t[:], in_=x_.t[:], func=AF.Square),
                     r=[x_.b], w=[sq_.b])
                pb = ps[4 + (g % 2)]

                def mm(e, sq_=sq_, pb=pb):
                    for kc in range(8):
                        ins = e.matmul(pb.t[:, :], lhsT=ones_b, rhs=sq_.t[:, kc, :], start=(kc == 0), stop=(kc == 7))
                    return ins
                S.op("pe", mm, r=[sq_.b, cb.b], w=[pb.b])
                S.op("act", lambda e, ln_=ln_, pb=pb: e.activation(out=ln_.t[:], in_=pb.t[:, :], func=AF.Ln,
                                                                  bias=EPS, scale=1.0 / 1024.0), r=[pb.b], w=[ln_.b])
                S.op("act", lambda e, ln_=ln_, rs_=rs_: e.activation(out=rs_.t[:], in_=ln_.t[:], func=AF.Exp, scale=-0.5),
                     r=[ln_.b], w=[rs_.b])
                S.op("dve", lambda e, x_=x_, rs_=rs_, g=g: e.tensor_tensor(
                    out=uT.t[:, :, g * 512:(g + 1) * 512], in0=x_.t[:],
                    in1=rs_.t[:].unsqueeze(1).to_broadcast([128, 8, 512]), op=ALU.mult),
                    r=[x_.b, rs_.b], w=[uTb[g]])

        def load_w(c0, ncols):
            i = cnt["w"] % 2
            cnt["w"] += 1
            wf, wb = Wf[i], Wb[i]
            S.dma("sp", lambda e: e.dma_start(out=wf.t[:, :, 0:ncols],
                                              in_=w_in[:, c0:c0 + ncols].rearrange("(kc p) n -> p kc n", p=128)), w=[wf.b])
            S.op("dve", lambda e: e.tensor_tensor(out=wb.t[:, :, 0:ncols], in0=wf.t[:, :, 0:ncols],
                                                  in1=gmix.t[:].unsqueeze(2).to_broadcast([128, 8, ncols]), op=ALU.mult),
                 r=[wf.b, gmix.b], w=[wb.b])
            return wb

        def evac(pb, np_, ncols, func, scale, dst_ap_fn):
            o = ostg[cnt["o"] % 4]
            k = cnt["o"]
            cnt["o"] += 1
            if func is None and (k % 2 == 1):
                if scale == 1.0:
                    S.op("dve", lambda e: e.tensor_copy(out=o.t[0:np_, 0:ncols], in_=pb.t[0:np_, 0:ncols]), r=[pb.b], w=[o.b])
                else:
                    S.op("dve", lambda e: e.tensor_scalar(out=o.t[0:np_, 0:ncols], in0=pb.t[0:np_, 0:ncols], scalar1=scale,
                                                          scalar2=None, op0=ALU.mult), r=[pb.b], w=[o.b])
            else:
                f = AF.Copy if func is None else func
                S.op("act", lambda e: e.activation(out=o.t[0:np_, 0:ncols], in_=pb.t[0:np_, 0:ncols], func=f, scale=scale),
                     r=[pb.b], w=[o.b])
            S.dma("pool", lambda e: e.dma_start(out=dst_ap_fn(), in_=o.t[0:np_, 0:ncols]), r=[o.b], w=[])
            convert_chunk()

        def nextps():
            pb = ps[cnt["ps"] % 4]
            cnt["ps"] += 1
            return pb

        def gemm_tm(c0, ncols, own, func, scale, dst, dcol0):
            wb = load_w(c0, ncols)
            tiles = [(t, t) for t in range(16 if own else 32)]
            for (pt, dt_) in tiles:
                pb = nextps()

                def mm(e, pt=pt, pb=pb):
                    for kc in range(8):
                        ins = e.matmul(pb.t[:, 0:ncols], lhsT=uT.t[:, kc, pt * 128:(pt + 1) * 128], rhs=wb.t[:, kc, 0:ncols],
                                       start=(kc == 0), stop=(kc == 7))
                    return ins
                S.op("pe", mm, r=[uTb[pt // 4], wb.b], w=[pb.b])
                evac(pb, 128, ncols, func, scale, lambda dt_=dt_: dst[dt_ * 128:(dt_ + 1) * 128, dcol0:dcol0 + ncols])

        def gemm_fm(c0, ncols, own, func, scale, dst, drow0):
            wb = load_w(c0, ncols)
            groups = [(g, g) for g in range(4 if own else 8)]
            for nci in range(ncols // 128):
                for (pg, dg) in groups:
                    pb = nextps()

                    def mm(e, pg=pg, pb=pb, nci=nci):
                        for kc in range(8):
                            ins = e.matmul(pb.t[:, :], lhsT=wb.t[:, kc, nci * 128:(nci + 1) * 128],
                                           rhs=uT.t[:, kc, pg * 512:(pg + 1) * 512], start=(kc == 0), stop=(kc == 7))
                        return ins
                    S.op("pe", mm, r=[uTb[pg], wb.b], w=[pb.b])
                    evac(pb, 128, 512, func, scale,
                         lambda dg=dg, nci=nci: dst[drow0 + nci * 128: drow0 + (nci + 1) * 128, dg * 512:(dg + 1) * 512])

        norm(xT_true, 8)
        gemm_tm(C_GK, 512, False, None, 1.0, s_kg, 0)
        gemm_tm(C_GV, 512, False, None, 1.0, s_vg, 0)
        gemm_tm(C_GV + 512, 512, False, None, 1.0, s_vg, 512)
        wb = load_w(C_GA, 16)
        for g in range(8):
            pb = nextps()

            def mm(e, g=g, pb=pb, wb=wb):
                for kc in range(8):
                    ins = e.matmul(pb.t[0:16, :], lhsT=wb.t[:, kc, 0:16], rhs=uT.t[:, kc, g * 512:(g + 1) * 512],
                                   start=(kc == 0), stop=(kc == 7))
                return ins
            S.op("pe", mm, r=[uTb[g], wb.b], w=[pb.b])
            S.op("act", lambda e, pb=pb: e.activation(out=ostg_f.t[:, :], in_=pb.t[0:16, :], func=AF.Copy), r=[pb.b], w=[ostg_f.b])
            S.dma("pool", lambda e, g=g: e.dma_start(out=s_aT[:, g * 512:(g + 1) * 512], in_=ostg_f.t[:, :]), r=[ostg_f.b], w=[])
        for q in range(2):
            gemm_fm(C_SK + 512 * q, 512, False, None, 1.0, s_ksb, 512 * q)
            gemm_tm(C_SV + 512 * q, 512, False, None, 1.0, s_vsb, 512 * q)
        norm(xT_own, 4)
        gemm_fm(C_GQ, 512, True, None, 128.0 ** -0.5, s_qg, 0)
        for q in range(2):
            gemm_fm(C_SQ + 512 * q, 512, True, None, 0.125, s_qsb, 512 * q)
        for q in range(2):
            gemm_tm(C_GR + 512 * q, 512, True, AF.Silu, 1.0, s_rg, 512 * q)
        for q in range(2):
            gemm_fm(C_GTA + 512 * q, 512, True, AF.Sigmoid, 1.0, s_sga, 512 * q)
        for q in range(2):
            gemm_fm(C_GTB + 512 * q, 512, True, AF.Sigmoid, 1.0, s_sgb, 512 * q)

    phaseA()
    S.barrier()
    A.reset(pmark)

    ogT = A.tile([128, 8, 2048], BF16, "ogT")
    osbT = A.tile([128, 8, 2048], BF16, "osbT")
    ogTb = [[Buf() for _ in range(16)] for _ in range(8)]
    osbTb = [[Buf() for _ in range(4)] for _ in range(8)]
    pmark2 = A.mark()

    def phaseB():
        kk = A.tile([128, 32, 256], BF16, "kk")
        kkb = [Buf() for _ in range(32)]
        vv = A.tile([128, 32, 512], BF16, "vv")
        qT = A.tile([128, 2, 2048], BF16, "qTg")
        aT = A.tile([16, 4096], F32, "aT")
        wup = A.tile([16, 512], F32, "wup")
        bab = A.tile([128, 512], F32, "bab")
        gob = A.tile([128, 1024], F32, "gob")
        obuf = A.tile([128, 16, 512], F32, "obuf")
        obb = [Buf() for _ in range(16)]
        gch = A.tile([128, 32, 4], F32, "gch")
        gchb = [Buf() for _ in range(32)]
        zt = A.tiles(2, [128, 256], F32, "zt")
        et = A.tiles(2, [128, 256], F32, "et")
        spt = A.tiles(2, [128, 256], F32, "spt")
        edt = A.tiles(2, [128, 256], F32, "edt")
        st_f = A.tiles(2, [128, 256], F32, "stf")
        st_b = [A.tiles(2, [128, 256], BF16, f"stb{h}") for h in range(2)]
        rt = A.tiles(2, [128, 512], BF16, "rt")
        ssq = A.tiles(2, [128, 4], F32, "ssq")
        junk = A.tile([128, 256], F32, "junk")
        t1 = A.tiles(2, [128, 512], F32, "t1")
        og = A.tiles(2, [128, 512], F32, "og")
        convert_hook = make_converter([None], [None], 0)
        S.dma("sp", lambda e: e.dma_start(out=aT.t[:], in_=s_aT[:, :]), w=[aT.b])
        S.dma("sp", lambda e: e.dma_start(out=wup.t[:], in_=w_a_up[:, :]), w=[wup.b])
        S.dma("sp", lambda e: e.dma_start(out=bab.t[:], in_=b_a_bc[:, :]), w=[bab.b])
        S.dma("sp", lambda e: e.dma_start(out=gob.t[:], in_=g_o_bc[:, :]), w=[gob.b])
        def passB(hp):
            c0 = hp * 256
            for q4 in range(4):
                S.dma("sp", lambda e, q4=q4: e.dma_start(
                    out=kk.t[:, q4 * 8:(q4 + 1) * 8, :],
                    in_=s_kg[q4 * 1024:(q4 + 1) * 1024, c0:c0 + 256].rearrange("(t p) n -> p t n", p=128)),
                    w=[kkb[t] for t in range(q4 * 8, q4 * 8 + 8)])
            S.dma("sp", lambda e: e.dma_start(out=vv.t[:], in_=s_vg[:, hp * 512:(hp + 1) * 512].rearrange("(t p) n -> p t n", p=128)),
                  w=[vv.b])
            S.dma("sp", lambda e: e.dma_start(out=qT.t[:], in_=s_qg[hp * 256:(hp + 1) * 256, :].rearrange("(h p) t -> p h t", p=128)),
                  w=[qT.b])
            for tt in range(32):
                i = tt % 2
                z_, e_, sp_, ed_ = zt[i], et[i], spt[i], edt[i]
                pz = ps[i]
                S.op("pe", lambda e, tt=tt, pz=pz: e.matmul(pz.t[:, 0:256], lhsT=aT.t[:, tt * 128:(tt + 1) * 128],
                                                             rhs=wup.t[:, c0:c0 + 256], start=True, stop=True),
                     r=[aT.b, wup.b], w=[pz.b])
                S.op("dve", lambda e, z_=z_, pz=pz: e.tensor_tensor(out=z_.t[:], in0=pz.t[:, 0:256], in1=bab.t[:, c0:c0 + 256], op=ALU.add),
                     r=[pz.b, bab.b], w=[z_.b])
                S.op("act", lambda e, z_=z_, e_=e_: e.activation(out=e_.t[:], in_=z_.t[:], func=AF.Exp, scale=-1.0), r=[z_.b], w=[e_.b])
                S.op("act", lambda e, sp_=sp_, e_=e_: e.activation(out=sp_.t[:], in_=e_.t[:], func=AF.Ln, bias=1.0, scale=1.0),
                     r=[e_.b], w=[sp_.b])
                pd = ps[2 + i]
                S.op("pe", lambda e, sp_=sp_, pd=pd: e.matmul(pd.t[:, 0:256], lhsT=trigla_f, rhs=sp_.t[:], start=True, stop=True),
                     r=[sp_.b, cf.b], w=[pd.b])
                S.op("act", lambda e, ed_=ed_, pd=pd: e.activation(out=ed_.t[:], in_=pd.t[:, 0:256], func=AF.Exp), r=[pd.b], w=[ed_.b])
                S.op("dve", lambda e, ed_=ed_, tt=tt: e.tensor_tensor(out=kk.t[:, tt, :], in0=kk.t[:, tt, :], in1=ed_.t[:], op=ALU.mult),
                     r=[ed_.b], w=[kkb[tt]])
                pg = ps[4 + i]

                def mmg(e, sp_=sp_, pg=pg):
                    for h in range(2):
                        ins = e.matmul(pg.t[:, 2 * h:2 * h + 2], lhsT=sp_.t[:, h * 128:(h + 1) * 128], rhs=chind_f, start=True, stop=True)
                    return ins
                S.op("pe", mmg, r=[sp_.b, cf.b], w=[pg.b])
                S.op("act", lambda e, pg=pg, tt=tt: e.activation(out=gch.t[:, tt, :], in_=pg.t[:, 0:4], func=AF.Exp), r=[pg.b], w=[gchb[tt]])
            def kvmm(c, h):
                tt, half = c // 2, c % 2
                pkv = ps[h * 2 + (c % 2)]
                S.op("pe", lambda e: e.matmul(
                    pkv.t[:, 0:256], lhsT=kk.t[64 * half:64 * half + 64, tt, h * 128:(h + 1) * 128],
                    rhs=vv.t[64 * half:64 * half + 64, tt, h * 256:(h + 1) * 256], start=True, stop=True),
                    r=[kkb[tt], vv.b], w=[pkv.b])

            def comb(c, h, otile, po, il, j, cand):
                r0 = 64 * (il % 2)
                fcol = (4 + j) if cand == 0 else j
                if cand == 0:
                    S.op("dve", lambda e: e.tensor_scalar(
                        out=obuf.t[r0:r0 + 64, otile, h * 256:(h + 1) * 256], in0=po.t[r0:r0 + 64, 0:256],
                        scalar1=flg.t[r0:r0 + 64, fcol:fcol + 1], scalar2=None, op0=ALU.mult),
                        r=[po.b, flg.b], w=[obb[otile]])
                else:
                    S.op("dve", lambda e: e.scalar_tensor_tensor(
                        out=obuf.t[r0:r0 + 64, otile, h * 256:(h + 1) * 256], in0=po.t[r0:r0 + 64, 0:256],
                        scalar=flg.t[r0:r0 + 64, fcol:fcol + 1], in1=obuf.t[r0:r0 + 64, otile, h * 256:(h + 1) * 256],
                        op0=ALU.mult, op1=ALU.add), r=[po.b, flg.b], w=[obb[otile]])

            pend = []
            for h in range(2):
                kvmm(0, h)
            for c in range(64):
                tt, half = c // 2, c % 2
                j, cand, il = c // 16, (c % 16) // 8, c % 8
                otile = 4 * j + il // 2
                for h in range(2):
                    pkv = ps[h * 2 + (c % 2)]
                    sf = st_f[h]
                    sb_ = st_b[h][c % 2]
                    if c == 0:
                        S.op("dve", lambda e, sf=sf, pkv=pkv: e.tensor_copy(out=sf.t[:], in_=pkv.t[:, 0:256]), r=[pkv.b], w=[sf.b])
                    else:
                        S.op("dve", lambda e, sf=sf, pkv=pkv, h=h, tt=tt, half=half: e.scalar_tensor_tensor(
                            out=sf.t[:], in0=sf.t[:], scalar=gch.t[:, tt, 2 * h + half:2 * h + half + 1], in1=pkv.t[:, 0:256],
                            op0=ALU.mult, op1=ALU.add), r=[pkv.b, gchb[tt]], w=[sf.b])
                    if c + 1 < 64:
                        kvmm(c + 1, h)
                    S.op("act", lambda e, sf=sf, sb_=sb_: e.activation(out=sb_.t[:], in_=sf.t[:], func=AF.Copy), r=[sf.b], w=[sb_.b])
                    po = ps[4 + h * 2 + (c % 2)]
                    S.op("pe", lambda e, h=h, otile=otile, po=po, sb_=sb_: e.matmul(
                        po.t[:, 0:256], lhsT=qT.t[:, h, otile * 128:(otile + 1) * 128], rhs=sb_.t[:], start=True, stop=True),
                        r=[qT.b, sb_.b], w=[po.b])
                    pend.append((c, h, otile, po, il, j, cand))
                    if len(pend) > 2:
                        comb(*pend.pop(0))
                convert_hook()
            while pend:
                comb(*pend.pop(0))
            for ot in range(16):
                i = ot % 2
                r_, q_, t1_, og_ = rt[i], ssq[i], t1[i], og[i]
                S.dma("sp", lambda e, r_=r_, ot=ot: e.dma_start(out=r_.t[:], in_=s_rg[ot * 128:(ot + 1) * 128, hp * 512:(hp + 1) * 512]), w=[r_.b])
                for h in range(2):
                    S.op("act", lambda e, h=h, ot=ot, q_=q_: e.activation(out=junk.t[:], in_=obuf.t[:, ot, h * 256:(h + 1) * 256], func=AF.Square,
                                                                         accum_out=q_.t[:, h:h + 1]), r=[obb[ot]], w=[junk.b, q_.b])
                S.op("act", lambda e, q_=q_: e.activation(out=q_.t[:, 2:4], in_=q_.t[:, 0:2], func=AF.Ln, bias=EPS, scale=1.0 / 256.0), r=[q_.b], w=[q_.b])
                S.op("act", lambda e, q_=q_: e.activation(out=q_.t[:, 0:2], in_=q_.t[:, 2:4], func=AF.Exp, scale=-0.5), r=[q_.b], w=[q_.b])
                for h in range(2):
                    S.op("dve", lambda e, h=h, ot=ot, q_=q_, t1_=t1_: e.scalar_tensor_tensor(
                        out=t1_.t[:, h * 256:(h + 1) * 256], in0=obuf.t[:, ot, h * 256:(h + 1) * 256], scalar=q_.t[:, h:h + 1],
                        in1=gob.t[:, hp * 512 + h * 256: hp * 512 + (h + 1) * 256], op0=ALU.mult, op1=ALU.mult),
                        r=[obb[ot], q_.b, gob.b], w=[t1_.b])
                S.op("dve", lambda e, t1_=t1_, r_=r_, og_=og_: e.tensor_tensor(out=og_.t[:], in0=t1_.t[:], in1=r_.t[:], op=ALU.mult),
                     r=[t1_.b, r_.b], w=[og_.b])
                pt_ = ps[6 + i]

                def tr(e, og_=og_, pt_=pt_):
                    for cch in range(4):
                        ins = e.transpose(pt_.t[:, cch * 128:(cch + 1) * 128], og_.t[:, cch * 128:(cch + 1) * 128], ident_f)
                    return ins
                S.op("pe", tr, r=[og_.b, cf.b], w=[pt_.b])
                S.op("act", lambda e, pt_=pt_, ot=ot: e.activation(
                    out=ogT.t[:, hp * 4:(hp + 1) * 4, ot * 128:(ot + 1) * 128],
                    in_=pt_.t[:, :].rearrange("p (c t) -> p c t", c=4), func=AF.Copy),
                    r=[pt_.b], w=[ogTb[hp * 4 + cch][ot] for cch in range(4)])
            if debug and hp == 1:
                S.dma("sp", lambda e: e.dma_start(out=dbg["obuf"][:, :], in_=obuf.t[:].rearrange("p a b -> p (a b)")), r=[], w=[])
                S.dma("sp", lambda e: e.dma_start(out=dbg["gch"][:, :], in_=gch.t[:].rearrange("p a b -> p (a b)")), r=[], w=[])
                S.dma("sp", lambda e: e.dma_start(out=dbg["kdec"][:, :], in_=kk.t[:].rearrange("p a b -> p (a b)")), r=[], w=[])
                S.dma("sp", lambda e: e.dma_start(out=dbg["aT"][:, :], in_=s_aT[:, :]), r=[], w=[])
                S.dma("sp", lambda e: e.dma_start(out=dbg["qg"][:, :], in_=s_qg[:, :]), r=[], w=[])
                S.dma("sp", lambda e: e.dma_start(out=dbg["rg"][:, :], in_=s_rg[:, :]), r=[], w=[])
                S.dma("sp", lambda e: e.dma_start(out=dbg["kg"][:, :], in_=s_kg[:, :]), r=[], w=[])
            S.barrier()

        passB(0)
        passB(1)
        S.barrier()

    phaseB()
    A.reset(pmark2)

    def phaseC():
        kT = A.tiles(2, [128, 4096], BF16, "kT")
        vp = A.tiles(2, [128, 32, 128], BF16, "vp")
        qT = A.tiles(2, [128, 2048], BF16, "qTs")
        msk = A.tile([128, 16, 512], BF16, "msk")
        NE, NSP, NEA, NW, NL = 7, 4, 3, 3, 5
        e_t = A.tiles(NE, [128, 1024], BF16, "e")
        sp_t = A.tiles(NSP, [128, 1024], BF16, "sp")
        ea_t = A.tiles(NEA, [128, 1024], BF16, "ea")
        w_t = A.tiles(NW, [128, 1024], BF16, "w")
        la_t = A.tiles(NL, [128, 512], BF16, "la")
        s01_t = A.tiles(2, [128, 512], BF16, "s01")
        cvfC = A.tiles(4, [128, 1024], F32, "cvfC")
        cvbC = A.tiles(4, [128, 1024], BF16, "cvbC")
        convertC = make_converter(cvfC, cvbC, NCV, pattern=("dve",))
        zp = [(ps[0], ps[1], psall[:, 0:1024]), (ps[2], ps[3], psall[:, 1024:2048])]
        apair = (ps[4], ps[5], psall[:, 2048:3072])
        ob = [ps[6], ps[7]]
        for q in range(4):
            S.dma("sp", lambda e, q=q: e.dma_start(out=msk.t[:, q * 4:(q + 1) * 4, :],
                                                   in_=masks_d[:, q * 2048:(q + 1) * 2048].rearrange("p (m t) -> p m t", m=4)), w=[msk.b])
        pairs = []
        jobn = 0
        for hp in range(8):
            for a in range(2):
                for j in range(4):
                    order = [(kt, True) for kt in range(8 * j + 7, 8 * j - 1, -1)] + [(kt, False) for kt in range(8 * j - 1, -1, -1)]
                    npair = len(order) // 2
                    for pi_ in range(npair):
                        pairs.append(dict(hp=hp, a=a, j=j, kts=(order[2 * pi_], order[2 * pi_ + 1]), first=(pi_ == 0), last=(pi_ == npair - 1),
                                          job=jobn))
                    jobn += 1
        npairs = len(pairs)
        for n, p in enumerate(pairs):
            p["n"] = n
        loaded = {}

        def load_pair(hp):
            i = hp % 2
            S.dma("sp", lambda e: e.dma_start(out=kT[i].t[:], in_=s_ksb[hp * 128:(hp + 1) * 128, :]), w=[kT[i].b])
            S.dma("sp", lambda e: e.dma_start(out=vp[i].t[:], in_=s_vsb[:, hp * 128:(hp + 1) * 128].rearrange("(t p) n -> p t n", p=128)),
                  w=[vp[i].b])
            S.dma("sp", lambda e: e.dma_start(out=qT[i].t[:], in_=s_qsb[hp * 128:(hp + 1) * 128, :]), w=[qT[i].b])
            loaded[hp] = True

        load_pair(0)

        def warm(e):
            for _ in range(64):
                ins = e.matmul(ps[7].t[:, :], lhsT=ones_b, rhs=cb.t[:, 0:512], start=True, stop=True)
            return ins
        S.op("pe", warm, r=[cb.b], w=[ps[7].b])

        def fZ(p):
            n, hp, a, j = p["n"], p["hp"], p["a"], p["j"]
            if p["first"] and a == 0 and j == 0 and hp + 1 < 8 and (hp + 1) not in loaded:
                load_pair(hp + 1)
            pi = hp % 2
            z0, z1, _ = zp[n % 2]

            def f(e):
                for zz, (kt, _) in zip((z0, z1), p["kts"]):
                    ins = e.matmul(zz.t[:, :], lhsT=kT[pi].t[64 * a:64 * a + 64, kt * 128:(kt + 1) * 128],
                                   rhs=qT[pi].t[64 * a:64 * a + 64, j * 512:(j + 1) * 512], start=True, stop=True)
                return ins
            S.op("pe", f, r=[kT[pi].b, qT[pi].b], w=[z0.b, z1.b])

        def fE(p):
            n, j = p["n"], p["j"]
            z0, z1, zap = zp[n % 2]
            e_ = e_t[n % NE]
            S.op("act", lambda e: e.activation(out=e_.t[:, :], in_=zap, func=AF.Exp), r=[z0.b, z1.b], w=[e_.b])
            for h_, (kt, cur) in enumerate(p["kts"]):
                if cur:
                    mi = (j % 2) * 8 + (kt - 8 * j)
                    S.op("dve", lambda e, h_=h_, mi=mi: e.tensor_tensor(out=e_.t[:, h_ * 512:(h_ + 1) * 512], in0=e_.t[:, h_ * 512:(h_ + 1) * 512],
                                                                       in1=msk.t[:, mi, :], op=ALU.mult), r=[msk.b], w=[e_.b])

        def fSP(p):
            n = p["n"]
            e_ = e_t[n % NE]
            sp_ = sp_t[n % NSP]
            S.op("act", lambda e: e.activation(out=sp_.t[:, :], in_=e_.t[:, :], func=AF.Ln, bias=1.0, scale=1.0), r=[e_.b], w=[sp_.b])
            if not p["last"]:
                la_new = la_t[n % NL]
                if p["first"]:
                    S.op("pool", lambda e: e.tensor_tensor(out=la_new.t[:, :], in0=sp_.t[:, 0:512], in1=sp_.t[:, 512:1024], op=ALU.add),
                         r=[sp_.b], w=[la_new.b])
                else:
                    la_old = la_t[(n - 1) % NL]
                    s01 = s01_t[n % 2]
                    S.op("pool", lambda e: e.tensor_tensor(out=s01.t[:, :], in0=sp_.t[:, 0:512], in1=sp_.t[:, 512:1024], op=ALU.add),
                         r=[sp_.b], w=[s01.b])
                    S.op("pool", lambda e: e.tensor_tensor(out=la_new.t[:, :], in0=la_old.t[:, :], in1=s01.t[:, :], op=ALU.add),
                         r=[s01.b, la_old.b], w=[la_new.b])

        def fA(p):
            n = p["n"]
            sp_ = sp_t[n % NSP]
            a0, a1, _ = apair
            la_old = None if p["first"] else la_t[(n - 1) % NL]

            def f(e):
                e.matmul(a0.t[:, :], lhsT=trisb_b, rhs=sp_.t[:, 0:512], start=True, stop=(la_old is None))
                if la_old is not None:
                    e.matmul(a0.t[:, :], lhsT=negones_b, rhs=la_old.t[:, :], start=False, stop=True)
                e.matmul(a1.t[:, :], lhsT=trisb_b, rhs=sp_.t[:, 512:1024], start=True, stop=False)
                ins = e.matmul(a1.t[:, :], lhsT=negones_b, rhs=sp_.t[:, 0:512], start=False, stop=(la_old is None))
                if la_old is not None:
                    ins = e.matmul(a1.t[:, :], lhsT=negones_b, rhs=la_old.t[:, :], start=False, stop=True)
                return ins
            S.op("pe", f, r=[sp_.b, cb.b] + ([] if la_old is None else [la_old.b]), w=[a0.b, a1.b])

        def fEA(p):
            n = p["n"]
            a0, a1, aap = apair
            ea_ = ea_t[n % NEA]
            S.op("act", lambda e: e.activation(out=ea_.t[:, :], in_=aap, func=AF.Exp), r=[a0.b, a1.b], w=[ea_.b])

        def fW(p):
            n = p["n"]
            e_, ea_, w_ = e_t[n % NE], ea_t[n % NEA], w_t[n % NW]
            S.op("dve", lambda e: e.tensor_tensor(out=w_.t[:, :], in0=e_.t[:, :], in1=ea_.t[:, :], op=ALU.mult), r=[e_.b, ea_.b], w=[w_.b])

        def fO(p):
            n, hp, a, j = p["n"], p["hp"], p["a"], p["j"]
            pi = hp % 2
            w_ = w_t[n % NW]
            o_ = ob[p["job"] % 2]

            def f(e):
                (kt0, _), (kt1, _) = p["kts"]
                e.matmul(o_.t[:, :], lhsT=vp[pi].t[:, kt0, :], rhs=w_.t[:, 0:512], start=p["first"], stop=False)
                return e.matmul(o_.t[:, :], lhsT=vp[pi].t[:, kt1, :], rhs=w_.t[:, 512:1024], start=False, stop=p["last"])
            S.op("pe", f, r=[vp[pi].b, w_.b], w=[o_.b])
            if p["last"]:
                S.op("act", lambda e: e.activation(out=osbT.t[64 * a:64 * a + 64, hp, j * 512:(j + 1) * 512],
                                                   in_=o_.t[64 * a:64 * a + 64, :], func=AF.Copy), r=[o_.b], w=[osbTb[hp][j]])

        stages = [(fEA, 4), (fZ, 0), (fE, 1), (fSP, 2), (fA, 3), (fW, 5), (fO, 6)]
        for s_ in range(npairs + 7):
            for fn, off in stages:
                if 0 <= s_ - off < npairs:
                    fn(pairs[s_ - off])
            if s_ >= 8 and cvstate["n"] < ((s_ - 8) * NCV) // (npairs - 60) + 1:
                convertC()
        while cvstate["n"] < NCV:
            convertC()
        S.barrier()

    phaseC()
    A.reset(pmark2)

    if debug:
        S.dma("sp", lambda e: e.dma_start(out=dbg["ogT"].rearrange("(c p) t -> p c t", p=128), in_=ogT.t[:]), r=[], w=[])
        S.dma("sp", lambda e: e.dma_start(out=dbg["osbT"].rearrange("(c p) t -> p c t", p=128), in_=osbT.t[:]), r=[], w=[])
        S.barrier()

    hbuf_holder = {}

    def load_w_bf(dst, src_bf, *_):
        S.dma("sp", lambda e: e.dma_start(out=dst.t[:], in_=src_bf.rearrange("(kc p) n -> p kc n", p=128)), w=[dst.b])

    def phaseD():
        hbuf = Tl(nc.alloc_sbuf_tensor_at("hbuf_al", [128, 16, 1024], F32, offset=pmark), "hbuf")
        hb = [Buf() for _ in range(16)]
        hbuf_holder["h"] = (hbuf, hb)
        m = A.mark()
        mixA = A.tile([128, 8, 2048], BF16, "mixA")
        mixb = [Buf() for _ in range(4)]
        Wga = A.tile([128, 8, 1024], BF16, "Wga")
        Wsb = A.tile([128, 8, 1024], BF16, "Wsb")
        stage = A.tiles(2, [128, 8, 128], F32, "wstg")
        sga = A.tiles(2, [128, 512], BF16, "sga")
        sgb = A.tiles(2, [128, 512], BF16, "sgb")
        m1 = A.tiles(2, [128, 512], F32, "m1")
        m2 = A.tiles(2, [128, 512], F32, "m2")
        xt = A.tiles(2, [128, 1024], F32, "xt")
        load_w_bf(Wga, s_wga)
        load_w_bf(Wsb, s_wsb)
        k = 0
        for g in range(4):
            for c in range(8):
                pa, pb_ = ps[(2 * k) % 4], ps[(2 * k + 1) % 4]
                m1_, m2_ = m1[k % 2], m2[k % 2]
                sa_, sb2_ = sga[k % 2], sgb[k % 2]
                k += 1
                S.dma("sp", lambda e, g=g, c=c, sa_=sa_: e.dma_start(out=sa_.t[:], in_=s_sga[c * 128:(c + 1) * 128, g * 512:(g + 1) * 512]),
                      w=[sa_.b])
                S.dma("sp", lambda e, g=g, c=c, sb2_=sb2_: e.dma_start(out=sb2_.t[:], in_=s_sgb[c * 128:(c + 1) * 128, g * 512:(g + 1) * 512]),
                      w=[sb2_.b])

                def mma(e, c=c, g=g, pa=pa):
                    for kc in range(8):
                        ins = e.matmul(pa.t[:, :], lhsT=Wga.t[:, kc, c * 128:(c + 1) * 128], rhs=ogT.t[:, kc, g * 512:(g + 1) * 512],
                                       start=(kc == 0), stop=(kc == 7))
                    return ins

                def mmb(e, c=c, g=g, pb_=pb_):
                    for kc in range(8):
                        ins = e.matmul(pb_.t[:, :], lhsT=Wsb.t[:, kc, c * 128:(c + 1) * 128], rhs=osbT.t[:, kc, g * 512:(g + 1) * 512],
                                       start=(kc == 0), stop=(kc == 7))
                    return ins
                S.op("pe", mma, r=[Wga.b] + [ogTb[kc][4 * g + q] for kc in range(8) for q in range(4)], w=[pa.b])
                S.op("pe", mmb, r=[Wsb.b] + [osbTb[kc][g] for kc in range(8)], w=[pb_.b])
                S.op("dve", lambda e, pa=pa, m1_=m1_, sa_=sa_: e.tensor_tensor(out=m1_.t[:], in0=pa.t[:, :], in1=sa_.t[:], op=ALU.mult),
                     r=[pa.b, sa_.b], w=[m1_.b])
                S.op("dve", lambda e, pb_=pb_, m2_=m2_, sb2_=sb2_: e.tensor_tensor(out=m2_.t[:], in0=pb_.t[:, :], in1=sb2_.t[:], op=ALU.mult),
                     r=[pb_.b, sb2_.b], w=[m2_.b])
                S.op("pool", lambda e, c=c, g=g, m1_=m1_, m2_=m2_: e.tensor_tensor(out=mixA.t[:, c, g * 512:(g + 1) * 512], in0=m1_.t[:],
                                                                                  in1=m2_.t[:], op=ALU.add),
                     r=[m1_.b, m2_.b], w=[mixb[g]])
        S.barrier()
        Wo = Wga
        load_w_bf(Wo, s_wo)
        for g in range(4):
            for tc in range(4):
                ot = 4 * g + tc
                x_ = xt[ot % 2]
                S.dma("sp", lambda e, x_=x_, ot=ot: e.dma_start(out=x_.t[:], in_=x_own[ot * 128:(ot + 1) * 128, :]), w=[x_.b])
                for nh in range(2):
                    ph = ps[4 + (2 * ot + nh) % 4]

                    def mmo(e, ot=ot, nh=nh, ph=ph):
                        for kc in range(8):
                            ins = e.matmul(ph.t[:, :], lhsT=mixA.t[:, kc, ot * 128:(ot + 1) * 128], rhs=Wo.t[:, kc, nh * 512:(nh + 1) * 512],
                                           start=(kc == 0), stop=(kc == 7))
                        return ins
                    S.op("pe", mmo, r=[mixb[g], Wo.b], w=[ph.b])
                    S.op("dve", lambda e, x_=x_, ot=ot, nh=nh, ph=ph: e.tensor_tensor(
                        out=hbuf.t[:, ot, nh * 512:(nh + 1) * 512], in0=ph.t[:, :], in1=x_.t[:, nh * 512:(nh + 1) * 512], op=ALU.add),
                        r=[ph.b, x_.b], w=[hb[ot]])
        S.barrier()
        A.reset(m)

    phaseD()
    hbuf, hb = hbuf_holder["h"]
    if debug:
        S.dma("sp", lambda e: e.dma_start(out=dbg["h1"].rearrange("(t p) n -> p t n", p=128), in_=hbuf.t[:]), r=[], w=[])
        S.barrier()

    eid_all = A.tile([128, 16, 128], I32, "eid_all")
    gate_all = A.tile([128, 16, 128], F32, "gate_all")
    eidb = [Buf() for _ in range(16)]
    gateb = [Buf() for _ in range(16)]
    pmark3 = A.mark()

    def make_rms(junk, sm):
        def rms(src_ap_fn, srcb, gtile, dst, dstb, stat0):
            S.op("act", lambda e: e.activation(out=junk.t[:], in_=src_ap_fn(), func=AF.Square, accum_out=sm.t[:, stat0:stat0 + 1]),
                 r=srcb, w=[junk.b, sm.b])
            S.op("act", lambda e: e.activation(out=sm.t[:, stat0 + 1:stat0 + 2], in_=sm.t[:, stat0:stat0 + 1], func=AF.Ln, bias=EPS,
                                               scale=1.0 / 1024.0), r=[sm.b], w=[sm.b])
            S.op("act", lambda e: e.activation(out=sm.t[:, stat0:stat0 + 1], in_=sm.t[:, stat0 + 1:stat0 + 2], func=AF.Exp, scale=-0.5),
                 r=[sm.b], w=[sm.b])
            S.op("dve", lambda e: e.scalar_tensor_tensor(out=dst.t[:], in0=src_ap_fn(), scalar=sm.t[:, stat0:stat0 + 1], in1=gtile.t[:],
                                                        op0=ALU.mult, op1=ALU.mult), r=srcb + [sm.b, gtile.b], w=dstb)
        return rms

    def transpose_to(dst, dstb, src):
        for hh in range(2):
            pt_ = ps[hh]

            def tr(e, hh=hh, pt_=pt_):
                for c in range(4):
                    ins = e.transpose(pt_.t[:, c * 128:(c + 1) * 128], src.t[:, (hh * 4 + c) * 128:(hh * 4 + c + 1) * 128], ident_f)
                return ins
            S.op("pe", tr, r=[src.b, cf.b], w=[pt_.b])
            S.op("act", lambda e, hh=hh, pt_=pt_: e.activation(out=dst.t[:, hh * 4:(hh + 1) * 4, :],
                                                              in_=pt_.t[:, :].rearrange("p (c t) -> p c t", c=4), func=AF.Copy),
                 r=[pt_.b], w=dstb)

    def phaseE1():
        Wpq = A.tile([128, 8, 2048], BF16, "Wpq")
        k12 = A.tile([128, 2, 1024], BF16, "k12")
        gff = A.tile([128, 1024], F32, "gff")
        m = A.mark()
        stage = A.tiles(2, [128, 8, 128], F32, "wstg")
        load_w_bf(Wpq, s_wpq)
        S.dma("sp", lambda e: e.dma_start(out=k12.t[:, 0, :], in_=s_k1[:, :]), w=[k12.b])
        S.dma("sp", lambda e: e.dma_start(out=k12.t[:, 1, :], in_=s_k2[:, :]), w=[k12.b])
        S.dma("sp", lambda e: e.dma_start(out=gff.t[:], in_=g_ffn_bc[:, :]), w=[gff.b])
        S.barrier()
        A.reset(m)
        xn2 = A.tiles(2, [128, 1024], F32, "xn")
        xnT2 = A.tiles(2, [128, 8, 128], BF16, "xnT")
        qTs2 = A.tiles(2, [128, 16, 128], BF16, "qTs")
        sc2 = A.tiles(2, [128, 16, 128], F32, "sc")
        v12 = A.tile([128, 16, 16], F32, "v12")
        i12 = A.tile([128, 16, 16], U32, "i12")
        i12f = A.tile([128, 16, 16], BF16, "i12h")
        cand = A.tile([128, 8, 256], F32, "cand")
        ts = A.tile([128, 8, 16], F32, "ts")
        eidf = A.tile([128, 128], F32, "eidf")
        junk = A.tile([128, 1024], F32, "junkE")
        posu = A.tile([128, 8, 16], U32, "posu")
        v12b = [Buf() for _ in range(16)]
        i12b = [Buf() for _ in range(16)]
        candb = [Buf() for _ in range(8)]
        tsb = [Buf() for _ in range(8)]
        posb = [Buf() for _ in range(8)]
        abu = A.tile([128, 2, 8, 16], U32, "abu")
        abf = A.tile([128, 2, 8, 16], BF16, "abf")
        oh = A.tile([128, 8, 16, 16], BF16, "oh")
        idab = A.tile([128, 2, 8, 16], F32, "idab")
        sm = A.tile([128, 64], F32, "smg")
        rms_l = [make_rms(junk, A.tile([128, 8], F32, f"smr{i}")) for i in range(2)]

        NCH = 4
        wks = A.tiles(NCH, [128, 256], F32, "wks")

        def top16_multi(chains, n):
            for c0_ in range(0, len(chains), NCH):
                grp = chains[c0_:c0_ + NCH]
                for stage_ in range(5):
                    for ci, (src, srcb, vout, iout, vb_, ib_) in enumerate(grp):
                        wk_ = wks[ci]
                        if stage_ == 0:
                            S.op("dve", lambda e, src=src, vout=vout: e.max(out=vout(0), in_=src()), r=srcb, w=[vb_])
                        elif stage_ == 1:
                            S.op("dve", lambda e, src=src, vout=vout, iout=iout: e.max_index(out=iout(0), in_max=vout(0), in_values=src()),
                                 r=srcb + [vb_], w=[ib_])
                        elif stage_ == 2:
                            S.op("dve", lambda e, src=src, vout=vout, wk_=wk_: e.match_replace(out=wk_.t[:, 0:n], in_to_replace=vout(0),
                                                                                              in_values=src(), imm_value=-1e30),
                                 r=srcb + [vb_], w=[wk_.b])
                        elif stage_ == 3:
                            S.op("dve", lambda e, vout=vout, wk_=wk_: e.max(out=vout(1), in_=wk_.t[:, 0:n]), r=[wk_.b], w=[vb_])
                        else:
                            S.op("dve", lambda e, vout=vout, iout=iout, wk_=wk_: e.max_index(out=iout(1), in_max=vout(1), in_values=wk_.t[:, 0:n]),
                                 r=[wk_.b, vb_], w=[ib_])

        def front(ot):
            hsrc = lambda ot=ot: hbuf.t[:, ot, :]
            xn, xnT, qTs, sc = xn2[ot % 2], xnT2[ot % 2], qTs2[ot % 2], sc2[ot % 2]
            rms_l[ot % 2](hsrc, [hb[ot]], gff, xn, [xn.b], 0)
            transpose_to(xnT, [xnT.b], xn)
            for qb in range(4):
                pq = ps[2 + qb]

                def mmq(e, qb=qb, pq=pq, xnT=xnT):
                    for c in range(4):
                        cc = qb * 4 + c
                        for kc in range(8):
                            ins = e.matmul(pq.t[:, c * 128:(c + 1) * 128], lhsT=Wpq.t[:, kc, cc * 128:(cc + 1) * 128], rhs=xnT.t[:, kc, :],
                                           start=(kc == 0), stop=(kc == 7), skip_group_check=True)
                    return ins
                S.op("pe", mmq, r=[Wpq.b, xnT.b], w=[pq.b])
                S.op("act", lambda e, qb=qb, pq=pq, qTs=qTs: e.activation(out=qTs.t[:, qb * 4:(qb + 1) * 4, :],
                                                                in_=pq.t[:, :].rearrange("p (c t) -> p c t", c=4), func=AF.Copy),
                     r=[pq.b], w=[qTs.b])
            for qb in range(4):
                pq = ps[2 + qb]

                def mms(e, qb=qb, pq=pq, qTs=qTs):
                    for c in range(4):
                        cc = qb * 4 + c
                        hh, half = cc // 2, cc % 2
                        ins = e.matmul(pq.t[:, c * 128:(c + 1) * 128], lhsT=qTs.t[:, cc, :], rhs=k12.t[:, half, hh * 128:(hh + 1) * 128],
                                       start=True, stop=True, skip_group_check=True)
                    return ins
                S.op("pe", mms, r=[qTs.b, k12.b], w=[pq.b])
                S.op("act", lambda e, qb=qb, pq=pq, sc=sc: e.activation(out=sc.t[:, qb * 4:(qb + 1) * 4, :],
                                                                in_=pq.t[:, :].rearrange("p (c t) -> p c t", c=4), func=AF.Copy),
                     r=[pq.b], w=[sc.b])
        def back(ot):
            sc = sc2[ot % 2]
            top16_multi([(lambda cc=cc, sc=sc: sc.t[:, cc, :], [sc.b], lambda r_, cc=cc: v12.t[:, cc, r_ * 8:(r_ + 1) * 8],
                          lambda r_, cc=cc: i12.t[:, cc, r_ * 8:(r_ + 1) * 8], v12b[cc], i12b[cc]) for cc in range(16)], 128)
            S.op("dve", lambda e: e.tensor_copy(out=i12f.t[:], in_=i12.t[:]), r=i12b, w=[i12f.b])
            for hh in range(8):
                S.op("dve", lambda e, hh=hh: e.tensor_tensor(
                    out=cand.t[:, hh, :].rearrange("p (a b) -> p a b", a=16),
                    in0=v12.t[:, 2 * hh, :].unsqueeze(2).to_broadcast([128, 16, 16]),
                    in1=v12.t[:, 2 * hh + 1, :].unsqueeze(1).to_broadcast([128, 16, 16]), op=ALU.add), r=[v12b[2 * hh], v12b[2 * hh + 1]], w=[candb[hh]])
            top16_multi([(lambda hh=hh: cand.t[:, hh, :], [candb[hh]], lambda r_, hh=hh: ts.t[:, hh, r_ * 8:(r_ + 1) * 8],
                          lambda r_, hh=hh: posu.t[:, hh, r_ * 8:(r_ + 1) * 8], tsb[hh], posb[hh]) for hh in range(8)], 256)
            S.op("dve", lambda e: e.tensor_single_scalar(out=abu.t[:, 0], in_=posu.t[:], scalar=4, op=ALU.logical_shift_right), r=posb, w=[abu.b])
            S.op("dve", lambda e: e.tensor_single_scalar(out=abu.t[:, 1], in_=posu.t[:], scalar=15, op=ALU.bitwise_and), r=posb, w=[abu.b])
            S.op("dve", lambda e: e.tensor_copy(out=abf.t[:], in_=abu.t[:]), r=[abu.b], w=[abf.b])
            for half in range(2):
                S.op("dve", lambda e, half=half: e.tensor_tensor(
                    out=oh.t[:], in0=abf.t[:, half].unsqueeze(3).to_broadcast([128, 8, 16, 16]),
                    in1=iota16_bt.t[:, :].unsqueeze(1).unsqueeze(1).to_broadcast([128, 8, 16, 16]), op=ALU.is_equal), r=[abf.b, iota16_bt.b], w=[oh.b])
                S.op("dve", lambda e, half=half: e.tensor_tensor(
                    out=oh.t[:], in0=oh.t[:],
                    in1=i12f.t[:].rearrange("p (h a) n -> p h a n", a=2)[:, :, half, :].unsqueeze(2).to_broadcast([128, 8, 16, 16]),
                    op=ALU.mult), r=[i12f.b], w=[oh.b])
                S.op("dve", lambda e, half=half: e.tensor_reduce(out=idab.t[:, half], in_=oh.t[:], axis=mybir.AxisListType.X, op=ALU.add),
                     r=[oh.b], w=[idab.b])
            S.op("dve", lambda e: e.scalar_tensor_tensor(out=eidf.t[:].rearrange("p (h k) -> p h k", h=8), in0=idab.t[:, 0], scalar=128.0,
                                                        in1=idab.t[:, 1], op0=ALU.mult, op1=ALU.add), r=[idab.b], w=[eidf.b])
            S.op("dve", lambda e, ot=ot: e.tensor_copy(out=eid_all.t[:, ot, :], in_=eidf.t[:]), r=[eidf.b], w=[eidb[ot]])
            if debug:
                S.dma("sp", lambda e, ot=ot: e.dma_start(out=dbg["eid"][ot * 128:(ot + 1) * 128, :], in_=eid_all.t[:, ot, :]), r=[eidb[ot]], w=[])
            for hh in range(8):
                S.op("dve", lambda e, hh=hh: e.tensor_scalar(out=sm.t[:, 8 + hh:9 + hh], in0=ts.t[:, hh, 0:1], scalar1=-1.0, scalar2=None,
                                                             op0=ALU.mult), r=[tsb[hh]], w=[sm.b])

        def gates(ot):
            for hh in range(8):
                S.op("act", lambda e, hh=hh, ot=ot: e.activation(out=gate_all.t[:, ot, hh * 16:(hh + 1) * 16], in_=ts.t[:, hh, :], func=AF.Exp,
                                                                 bias=sm.t[:, 8 + hh:9 + hh], scale=1.0, accum_out=sm.t[:, 16 + hh:17 + hh]),
                     r=[tsb[hh], sm.b], w=[gateb[ot], sm.b])
            S.op("dve", lambda e: e.reciprocal(out=sm.t[:, 24:32], in_=sm.t[:, 16:24]), r=[sm.b], w=[sm.b])
            S.op("dve", lambda e, ot=ot: e.tensor_tensor(
                out=gate_all.t[:, ot, :].rearrange("p (h k) -> p h k", h=8), in0=gate_all.t[:, ot, :].rearrange("p (h k) -> p h k", h=8),
                in1=sm.t[:, 24:32].unsqueeze(2).to_broadcast([128, 8, 16]), op=ALU.mult), r=[sm.b], w=[gateb[ot]])
        front(0)
        for ot in range(16):
            if ot >= 1:
                gates(ot - 1)
            if ot + 1 < 16:
                front(ot + 1)
            back(ot)
        gates(15)
        S.barrier()

    phaseE1()
    A.reset(pmark3)

    def phaseE2():
        GK = 4
        NB = 26
        gff = A.tile([128, 1024], F32, "gff2")
        xn = A.tile([128, 1024], BF16, "xn2")
        junk = A.tile([128, 1024], F32, "junkE2")
        junkb = A.tile([128, 1024], BF16, "junkE2b")
        sm = A.tile([128, 64], F32, "sm2")
        hd = A.tile([128, 128], F32, "hd")
        ga_ = A.tile([128, 128], F32, "ga")
        tq = A.tiles(4, [128, 128], F32, "tq")
        uv = A.tiles(NB, [128, 2048], BF16, "uv")
        dg = A.tiles(6, [128, 128], BF16, "dg")
        S.dma("sp", lambda e: e.dma_start(out=gff.t[:], in_=g_ffn_bc[:, :]), w=[gff.b])
        rms = make_rms(junk, sm)
        NG = 128 // GK
        hdb = [Buf() for _ in range(NG)]
        gab = [Buf() for _ in range(NG)]
        tqb = [[Buf() for _ in range(NG)] for _ in range(4)]
        cnt = {"u": 0, "d": 0}
        t0, t1_, t2, t3 = tq
        for ot in range(16):
            hsrc = lambda ot=ot: hbuf.t[:, ot, :]
            rms(hsrc, [hb[ot]], gff, xn, [xn.b], 0)
            held = {}
            pacc = (ps[4 + 2 * (ot % 2)], ps[5 + 2 * (ot % 2)])

            def post(g, ot=ot, pacc=pacc):
                sl = slice(g * GK, (g + 1) * GK)
                S.op("dve", lambda e: e.tensor_tensor(out=t0.t[:, sl], in0=t3.t[:, sl], in1=hd.t[:, sl], op=ALU.mult),
                     r=[tqb[3][g], hdb[g]], w=[tqb[0][g]])
                S.op("dve", lambda e: e.tensor_tensor(out=ga_.t[:, sl], in0=t0.t[:, sl], in1=gate_all.t[:, ot, sl], op=ALU.mult),
                     r=[tqb[0][g], gateb[ot]], w=[gab[g]])
                for k in range(GK):
                    kk_ = g * GK + k
                    b_ = held[kk_]
                    d_ = dg[cnt["d"] % 6]
                    cnt["d"] += 1
                    S.op("act", lambda e, d_=d_, kk_=kk_: e.activation(out=d_.t[:], in_=ident_f, func=AF.Copy, scale=ga_.t[:, kk_:kk_ + 1]),
                         r=[gab[g], cf.b], w=[d_.b])

                    def mmv(e, d_=d_, b_=b_, kk_=kk_):
                        e.matmul(pacc[0].t[:, :], lhsT=d_.t[:], rhs=b_.t[:, 1024:1536], start=(kk_ == 0), stop=(kk_ == 127))
                        return e.matmul(pacc[1].t[:, :], lhsT=d_.t[:], rhs=b_.t[:, 1536:2048], start=(kk_ == 0), stop=(kk_ == 127))
                    S.op("pe", mmv, r=[d_.b, b_.b], w=[pacc[0].b, pacc[1].b])

            for g in range(NG):
                sl = slice(g * GK, (g + 1) * GK)
                for k in range(GK):
                    kk_ = g * GK + k
                    b_ = uv[cnt["u"] % NB]
                    cnt["u"] += 1
                    held[kk_] = b_
                    S.dma("pool", lambda e, b_=b_, kk_=kk_, ot=ot: e.indirect_dma_start(
                        out=b_.t[:, :], out_offset=None, in_=s_uvb[:, :],
                        in_offset=bass.IndirectOffsetOnAxis(ap=eid_all.t[:, ot, kk_:kk_ + 1], axis=0)), r=[eidb[ot]], w=[b_.b])
                    S.op("dve", lambda e, b_=b_, kk_=kk_: e.scalar_tensor_tensor(
                        out=junkb.t[:], in0=b_.t[:, 0:1024], scalar=1.0, in1=xn.t[:], op0=ALU.mult, op1=ALU.mult,
                        accum_out=hd.t[:, kk_:kk_ + 1]), r=[b_.b, xn.b], w=[junkb.b, hdb[g]])
                S.op("act", lambda e, sl=sl: e.activation(out=t1_.t[:, sl], in_=hd.t[:, sl], func=AF.Square, scale=0.21145921592590275),
                     r=[hdb[g]], w=[tqb[1][g]])
                if g >= 1:
                    post(g - 1)
                S.op("dve", lambda e, sl=sl: e.scalar_tensor_tensor(out=t2.t[:, sl], in0=t1_.t[:, sl], scalar=1.0, in1=hd.t[:, sl],
                                                                    op0=ALU.add, op1=ALU.mult),
                     r=[tqb[1][g], hdb[g]], w=[tqb[2][g]])
                S.op("act", lambda e, sl=sl: e.activation(out=t3.t[:, sl], in_=t2.t[:, sl], func=AF.Sigmoid, scale=1.5957691216057308),
                     r=[tqb[2][g]], w=[tqb[3][g]])
            post(NG - 1)
            for nh in range(2):
                S.op("dve", lambda e, ot=ot, nh=nh, pacc=pacc: e.tensor_tensor(
                    out=hbuf.t[:, ot, nh * 512:(nh + 1) * 512], in0=pacc[nh].t[:, :], in1=hbuf.t[:, ot, nh * 512:(nh + 1) * 512], op=ALU.add),
                    r=[pacc[nh].b], w=[hb[ot]])
            if debug:
                S.dma("sp", lambda e, ot=ot: e.dma_start(out=dbg["h2"][ot * 128:(ot + 1) * 128, :], in_=hbuf.t[:, ot, :]), r=[hb[ot]], w=[])
        S.barrier()

    phaseE2()
    A.reset(pmark2)

    def phaseF():
        Wpg = A.tile([128, 8, 1024], BF16, "Wpg")
        Wpl = A.tile([128, 2, 1024], BF16, "Wpl")
        pTb = A.tile([128, 2, 2048], BF16, "pTb")
        gpl = A.tile([128, 1024], F32, "gpl")
        gfi = A.tile([128, 1024], F32, "gfi")
        stage = A.tiles(2, [128, 8, 128], F32, "wstg")
        stage2 = A.tiles(2, [128, 2, 512], F32, "wstg2")
        load_w_bf(Wpg, s_wpg)
        load_w_bf(Wpl, s_wpl)
        S.dma("sp", lambda e: e.dma_start(out=pTb.t[:], in_=s_pT.rearrange("(kc p) n -> p kc n", p=128)), w=[pTb.b])
        S.dma("sp", lambda e: e.dma_start(out=gpl.t[:], in_=g_ple_bc[:, :]), w=[gpl.b])
        S.dma("sp", lambda e: e.dma_start(out=gfi.t[:], in_=g_fin_bc[:, :]), w=[gfi.b])
        xn = A.tiles(2, [128, 1024], F32, "xnF")
        xnT = A.tiles(2, [128, 8, 128], BF16, "xnTF")
        junk = A.tile([128, 1024], F32, "junkF")
        sig = A.tiles(2, [128, 1024], F32, "sigF")
        tmp = A.tiles(2, [128, 1024], F32, "tmpF")
        outt = A.tiles(2, [128, 1024], F32, "outF")
        sm = A.tile([128, 64], F32, "smF")
        rms1 = make_rms(junk, sm)
        rms2 = make_rms(A.tile([128, 1024], F32, "junkF2"), A.tile([128, 8], F32, "smF2"))

        def front(ot):
            i = ot % 2
            hsrc = lambda ot=ot: hbuf.t[:, ot, :]
            rms1(hsrc, [hb[ot]], gpl, xn[i], [xn[i].b], 0)
            transpose_to(xnT[i], [xnT[i].b], xn[i])
            for nh in range(2):
                pg_, pp_ = ps[2 + nh], ps[4 + 2 * i + nh]

                def mmg(e, nh=nh, pg_=pg_, i=i):
                    for kc in range(8):
                        ins = e.matmul(pg_.t[:, :], lhsT=xnT[i].t[:, kc, :], rhs=Wpg.t[:, kc, nh * 512:(nh + 1) * 512], start=(kc == 0), stop=(kc == 7))
                    return ins

                def mmp(e, nh=nh, pp_=pp_, ot=ot):
                    for kc in range(2):
                        ins = e.matmul(pp_.t[:, :], lhsT=pTb.t[:, kc, ot * 128:(ot + 1) * 128], rhs=Wpl.t[:, kc, nh * 512:(nh + 1) * 512],
                                       start=(kc == 0), stop=(kc == 1))
                    return ins
                S.op("pe", mmg, r=[xnT[i].b, Wpg.b], w=[pg_.b])
                S.op("pe", mmp, r=[pTb.b, Wpl.b], w=[pp_.b])
                S.op("act", lambda e, nh=nh, pg_=pg_, i=i: e.activation(out=sig[i].t[:, nh * 512:(nh + 1) * 512], in_=pg_.t[:, :], func=AF.Sigmoid),
                     r=[pg_.b], w=[sig[i].b])

        def back(ot):
            i = ot % 2
            hsrc = lambda ot=ot: hbuf.t[:, ot, :]
            for nh in range(2):
                pp_ = ps[4 + 2 * i + nh]
                S.op("dve", lambda e, nh=nh, pp_=pp_, i=i: e.tensor_tensor(out=tmp[i].t[:, nh * 512:(nh + 1) * 512], in0=pp_.t[:, :],
                                                                          in1=sig[i].t[:, nh * 512:(nh + 1) * 512], op=ALU.mult),
                     r=[pp_.b, sig[i].b], w=[tmp[i].b])
            S.op("dve", lambda e, ot=ot, i=i: e.tensor_tensor(out=hbuf.t[:, ot, :], in0=hbuf.t[:, ot, :], in1=tmp[i].t[:], op=ALU.add),
                 r=[tmp[i].b], w=[hb[ot]])
            o_ = outt[i]
            rms2(hsrc, [hb[ot]], gfi, o_, [o_.b], 0)
            S.dma("sp", lambda e, ot=ot, o_=o_: e.dma_start(out=out_d[ot * 128:(ot + 1) * 128, :], in_=o_.t[:]), r=[o_.b], w=[])

        front(0)
        for ot in range(16):
            if ot + 1 < 16:
                front(ot + 1)
            back(ot)

    phaseF()
    S.barrier()
    S.emit()
    return nc


def _consts():
    c = np.zeros((128, 6 * 128 + 2 + 16), np.float32)
    j = np.arange(128)[:, None]
    s = np.arange(128)[None, :]
    c[:, 0:128] = np.eye(128, dtype=np.float32)
    c[:, 128:256] = 1.0
    c[:, 256:384] = np.where(j >= s, -1.0, 0.0)
    c[:, 384:512] = -1.0
    c[:, 512:640] = np.where((j > s) & ((j // 64) == (s // 64)), -1.0 / 16.0, 0.0)
    c[:, 640:768] = np.where(j < s, 1.0, 0.0)
    c[:, 768] = np.where(np.arange(128) < 64, -1.0 / 16.0, 0.0)
    c[:, 769] = np.where(np.arange(128) >= 64, -1.0 / 16.0, 0.0)
    c[:, 770:786] = np.arange(16, dtype=np.float32)[None, :]
    return c


def _masks(hf):
    sp = np.arange(128)[:, None]
    t = np.arange(512)[None, :]
    qb, tq = t // 128, t % 128
    m = np.zeros((128, 2, 8, 512), np.float32)
    for par in range(2):
        delta = (par + hf) % 2
        for i in range(4):
            bt = ((qb > i) | ((qb == i) & (sp < tq))).astype(np.float32)
            if delta == 0:
                m[:, par, i] = bt
                m[:, par, 4 + i] = 0.0
            else:
                m[:, par, i] = 1.0
                m[:, par, 4 + i] = bt
    return np.ascontiguousarray(m.reshape(128, 16 * 512)).astype(ml_dtypes.bfloat16)


def _own_groups(hf):
    delta = [(j + hf) % 2 for j in range(4)]
    return [2 * j + delta[j] for j in range(4)], delta


def _core_inputs(c, inp, shared):
    b, hf = c // 2, c % 2
    own, delta = _own_groups(hf)
    x = inp["x"][b]
    xT = np.ascontiguousarray(x.T)
    xg = x.reshape(8, 512, 1024)
    x_own = np.ascontiguousarray(xg[own].reshape(2048, 1024))
    p_own = inp["p"][0, b].reshape(8, 512, 256)[own].reshape(2048, 256)
    flags = np.zeros((128, 8), np.float32)
    for j in range(4):
        flags[:, j] = float(delta[j])
        flags[:, 4 + j] = 1.0 - float(delta[j])
    d = dict(shared)
    d.update({
        "xT_true": xT,
        "xT_own": np.ascontiguousarray(x_own.T),
        "masks": _masks(hf),
        "x_own": x_own,
        "pT_own": np.ascontiguousarray(p_own.T),
        "flags": flags,
    })
    return d


def _shared_inputs(inp):
    f = np.float32
    bc = lambda v: np.ascontiguousarray(np.broadcast_to(np.asarray(v, f).reshape(1, -1), (128, v.size)))
    return {
        "w_in": np.ascontiguousarray(inp["w_in"][0], f),
        "w_a_up": np.ascontiguousarray(inp["w_gla_a_up"][0], f),
        "b_a_bc": bc(inp["b_gla_a"][0]),
        "g_mix_pk": np.ascontiguousarray(np.asarray(inp["g_mix"][0], f).reshape(8, 128).T),
        "g_o_bc": bc(inp["g_gla_o"][0]),
        "w_gla_out": np.ascontiguousarray(inp["w_gla_out"][0], f),
        "w_sb_out": np.ascontiguousarray(inp["w_sb_out"][0], f),
        "w_o": np.ascontiguousarray(inp["w_o"][0], f),
        "g_ffn_bc": bc(inp["g_ffn"][0]),
        "w_pq": np.ascontiguousarray(inp["w_peer_q"][0], f),
        "k1T": np.ascontiguousarray(np.asarray(inp["peer_k1"][0], f).transpose(2, 0, 1).reshape(128, 1024)),
        "k2T": np.ascontiguousarray(np.asarray(inp["peer_k2"][0], f).transpose(2, 0, 1).reshape(128, 1024)),
        "peer_uv": np.ascontiguousarray(np.concatenate([np.asarray(inp["peer_u"][0], f), np.asarray(inp["peer_v"][0], f)], axis=1)),
        "g_ple_bc": bc(inp["g_ple"][0]),
        "w_pg": np.ascontiguousarray(inp["w_ple_gate"][0], f),
        "w_ple": np.ascontiguousarray(inp["w_ple"][0], f),
        "g_fin_bc": bc(inp["g_final"]),
        "consts": _consts(),
    }


def kernel(**inputs):
    inp = {k: np.asarray(v) for k, v in inputs.items()}
    shared = _shared_inputs(inp)
    in_maps = [_core_inputs(c, inp, shared) for c in range(8)]
    nc = build()
    res = run_bass_kernel_spmd(nc, in_maps, core_ids=list(range(8)))
    out = np.zeros((4, 8, 512, 1024), np.float32)
    for c in range(8):
        b, hf = c // 2, c % 2
        own, _ = _own_groups(hf)
        o = np.asarray(res.results[c]["out"]).reshape(4, 512, 1024)
        for j in range(4):
            out[b, own[j]] = o[j]
    return out.reshape(4, 4096, 1024)
```
